# Optimizing a Trainium2 kernel written in Bass

```python
import math
import jax, jax.numpy as jnp
from jax import lax
import numpy as np

D_MODEL = 1024
BATCH = 4
SEQ = 8192
DEPTH = 1

CHUNK = 64
Q_BLOCK = 128
ATT_HEADS = 8
ATT_HEAD_DIM = 64
ATT_WIDTH = ATT_HEADS * ATT_HEAD_DIM
IDX_HEADS = 8
IDX_DIM = 64
TOPK_MAX = 256
REL_BUCKETS = 32
REL_MAX_DIST = 1024
CONV_CH = 512
CONV_WIDTH = 31
N_BRANCH = 2
PEER_HEADS = 8
N_KEYS = 128
N_EXPERTS = N_KEYS * N_KEYS
PEER_QDIM = 256
PEER_HALF = PEER_QDIM // 2
PEER_TOPK = 16
PEER_TOKEN_BLOCK = 128
PLE_DIM = 256
EPS = 1e-6

IN_WIDTHS = (ATT_WIDTH, ATT_WIDTH, ATT_WIDTH, IDX_HEADS * IDX_DIM, IDX_DIM, IDX_HEADS, 2 * CONV_CH, N_BRANCH * D_MODEL)
IN_TOTAL = 3 * ATT_WIDTH + IDX_HEADS * IDX_DIM + IDX_DIM + IDX_HEADS + 2 * CONV_CH + N_BRANCH * D_MODEL

kernel_name = 'hybrid_dsa_conformer_peer_block'


def _split_points():
    pts, acc = [], 0
    for w in IN_WIDTHS[:-1]:
        acc += w
        pts.append(acc)
    return pts


def rms_norm(x, g):
    x32 = x.astype(jnp.float32)
    y = x32 * lax.rsqrt(jnp.mean(x32 * x32, axis=-1, keepdims=True) + EPS)
    return (y * g.astype(jnp.float32)).astype(x.dtype)


def layer_norm(x, g, b):
    x32 = x.astype(jnp.float32)
    mu = jnp.mean(x32, axis=-1, keepdims=True)
    xc = x32 - mu
    y = xc * lax.rsqrt(jnp.mean(xc * xc, axis=-1, keepdims=True) + EPS)
    return (y * g.astype(jnp.float32) + b.astype(jnp.float32)).astype(x.dtype)


def t5_bucket(rel):
    half = REL_BUCKETS // 2
    max_exact = half // 2
    ret = jnp.where(rel > 0, half, 0)
    n = jnp.abs(rel)
    nf = jnp.maximum(n, 1).astype(jnp.float32)
    large = max_exact + (jnp.log(nf / max_exact) / math.log(REL_MAX_DIST / max_exact) * (half - max_exact)).astype(jnp.int32)
    large = jnp.minimum(large, half - 1)
    return ret + jnp.where(n < max_exact, n, large)


def dsa_attention(q, k, v, qi, ki, wi, rel_bias):
    B, S = q.shape[0], q.shape[1]
    topk = min(TOPK_MAX, S // 4)
    nb = S // Q_BLOCK
    key_chunk = jnp.arange(S, dtype=jnp.int32) // CHUNK
    scale = ATT_HEAD_DIM ** -0.5
    idx_scale = (IDX_DIM ** -0.5) * (IDX_HEADS ** -0.5)

    def blockify(a):
        return jnp.moveaxis(a.reshape((B, nb, Q_BLOCK) + a.shape[2:]), 1, 0)

    def one_block(args):
        qb, qib, wib, qpos = args
        qchunk = qpos // CHUNK
        dots = jax.nn.relu(jnp.einsum('bqhd,bsd->bqhs', qib, ki))
        score = jnp.einsum('bqhs,bqh->bqs', dots, wib) * idx_scale
        visible = key_chunk[None, :] <= qchunk[:, None]
        score = jnp.where(visible[None], score, -jnp.inf)
        _, sel = lax.top_k(score, topk)
        kg = jax.vmap(lambda kk, ii: kk[ii])(k, sel)
        vg = jax.vmap(lambda vv, ii: vv[ii])(v, sel)
        logits = jnp.einsum('bqhd,bqkhd->bqhk', qb, kg).astype(jnp.float32) * scale
        bias = rel_bias[t5_bucket(sel - qpos[None, :, None])]
        logits = logits + jnp.transpose(bias, (0, 1, 3, 2)).astype(jnp.float32)
        ok = (sel // CHUNK) <= qchunk[None, :, None]
        logits = jnp.where(ok[:, :, None, :], logits, -jnp.inf)
        probs = jax.nn.softmax(logits, axis=-1).astype(vg.dtype)
        return jnp.einsum('bqhk,bqkhd->bqhd', probs, vg)

    qpos_blocks = jnp.arange(S, dtype=jnp.int32).reshape(nb, Q_BLOCK)
    out = lax.map(one_block, (blockify(q), blockify(qi), blockify(wi), qpos_blocks))
    return jnp.moveaxis(out, 0, 1).reshape(B, S, ATT_WIDTH)


def conformer_conv(glu_in, conv_w, conv_b, ln_g, ln_b, w_proj):
    a, g = jnp.split(glu_in, 2, axis=-1)
    u = a * jax.nn.sigmoid(g)
    y = lax.conv_general_dilated(u, conv_w, window_strides=(1,), padding=[(CONV_WIDTH - 1, 0)],
                                 dimension_numbers=('NWC', 'WIO', 'NWC'), feature_group_count=CONV_CH)
    y = layer_norm(y + conv_b, ln_g, ln_b)
    return jax.nn.silu(y) @ w_proj


def peer(h, w_query, sub_keys, expert_u, expert_v):
    B, S, D = h.shape
    q = (h @ w_query).reshape(B, S, PEER_HEADS, 2, PEER_HALF)
    sc = jnp.einsum('bshcd,hckd->bshck', q, sub_keys)
    sv, si = lax.top_k(sc, PEER_TOPK)
    n_cand = PEER_TOPK * PEER_TOPK
    cand_s = (sv[..., 0, :, None] + sv[..., 1, None, :]).reshape(B, S, PEER_HEADS, n_cand)
    cand_i = (si[..., 0, :, None] * N_KEYS + si[..., 1, None, :]).reshape(B, S, PEER_HEADS, n_cand)
    top_s, top_j = lax.top_k(cand_s, PEER_TOPK)
    eidx = jnp.take_along_axis(cand_i, top_j, axis=-1)
    gate = jax.nn.softmax(top_s.astype(jnp.float32), axis=-1).astype(h.dtype)
    n_sel = PEER_HEADS * PEER_TOPK
    nb = (B * S) // PEER_TOKEN_BLOCK
    hb = h.reshape(nb, PEER_TOKEN_BLOCK, D)
    eb = eidx.reshape(nb, PEER_TOKEN_BLOCK, n_sel)
    gb = gate.reshape(nb, PEER_TOKEN_BLOCK, n_sel)

    def one_block(args):
        xt, et, gt = args
        u = expert_u[et]
        v = expert_v[et]
        act = jax.nn.gelu(jnp.einsum('tnd,td->tn', u, xt)) * gt
        return jnp.einsum('tn,tnd->td', act, v)

    return lax.map(one_block, (hb, eb, gb)).reshape(B, S, D)


def setup_inputs(seed: int = 0) -> dict:
    key = jax.random.key(seed)
    ks = jax.random.split(key, 24)
    f = jnp.float32

    def nrm(k, shape, scale):
        return jax.random.normal(k, shape, f) * scale

    def gain(k, shape):
        return 1.0 + 0.05 * jax.random.normal(k, shape, f)

    return {
        'x': nrm(ks[0], (BATCH, SEQ, D_MODEL), 1.0),
        'p': nrm(ks[1], (DEPTH, BATCH, SEQ, PLE_DIM), 1.0),
        'rel_bias': nrm(ks[2], (REL_BUCKETS, ATT_HEADS), 0.1),
        'attn_norm_g': gain(ks[3], (DEPTH, D_MODEL)),
        'w_in': nrm(ks[4], (DEPTH, D_MODEL, IN_TOTAL), D_MODEL ** -0.5),
        'b_gate': nrm(ks[5], (DEPTH, N_BRANCH * D_MODEL), 0.02),
        'q_norm_g': gain(ks[6], (DEPTH, ATT_HEAD_DIM)),
        'k_norm_g': gain(ks[7], (DEPTH, ATT_HEAD_DIM)),
        'w_att_out': nrm(ks[8], (DEPTH, ATT_WIDTH, D_MODEL), ATT_WIDTH ** -0.5),
        'conv_w': nrm(ks[9], (DEPTH, CONV_WIDTH, 1, CONV_CH), CONV_WIDTH ** -0.5),
        'conv_b': nrm(ks[10], (DEPTH, CONV_CH), 0.02),
        'conv_ln_g': gain(ks[11], (DEPTH, CONV_CH)),
        'conv_ln_b': nrm(ks[12], (DEPTH, CONV_CH), 0.02),
        'w_conv_out': nrm(ks[13], (DEPTH, CONV_CH, D_MODEL), CONV_CH ** -0.5),
        'w_out': nrm(ks[14], (DEPTH, D_MODEL, D_MODEL), D_MODEL ** -0.5),
        'ffn_norm_g': gain(ks[15], (DEPTH, D_MODEL)),
        'w_peer_q': nrm(ks[16], (DEPTH, D_MODEL, PEER_HEADS * PEER_QDIM), D_MODEL ** -0.5),
        'peer_sub_keys': nrm(ks[17], (DEPTH, PEER_HEADS, 2, N_KEYS, PEER_HALF), PEER_HALF ** -0.5),
        'peer_u': nrm(ks[18], (DEPTH, N_EXPERTS, D_MODEL), D_MODEL ** -0.5),
        'peer_v': nrm(ks[19], (DEPTH, N_EXPERTS, D_MODEL), PEER_HEADS ** -0.5),
        'ple_norm_g': gain(ks[20], (DEPTH, D_MODEL)),
        'w_ple_gate': nrm(ks[21], (DEPTH, D_MODEL, D_MODEL), D_MODEL ** -0.5),
        'w_ple_proj': nrm(ks[22], (DEPTH, PLE_DIM, D_MODEL), PLE_DIM ** -0.5),
    }


def reference(x, p, rel_bias, attn_norm_g, w_in, b_gate, q_norm_g, k_norm_g, w_att_out,
              conv_w, conv_b, conv_ln_g, conv_ln_b, w_conv_out, w_out, ffn_norm_g,
              w_peer_q, peer_sub_keys, peer_u, peer_v, ple_norm_g, w_ple_gate, w_ple_proj):
    B, S, D = x.shape
    for i in range(DEPTH):
        h = rms_norm(x, attn_norm_g[i])
        proj = h @ w_in[i]
        q, k, v, qi, ki, wi, glu_in, gates = jnp.split(proj, _split_points(), axis=-1)
        q = rms_norm(q.reshape(B, S, ATT_HEADS, ATT_HEAD_DIM), q_norm_g[i])
        k = rms_norm(k.reshape(B, S, ATT_HEADS, ATT_HEAD_DIM), k_norm_g[i])
        v = v.reshape(B, S, ATT_HEADS, ATT_HEAD_DIM)
        qi = qi.reshape(B, S, IDX_HEADS, IDX_DIM)
        y_att = dsa_attention(q, k, v, qi, ki, wi, rel_bias) @ w_att_out[i]
        y_conv = conformer_conv(glu_in, conv_w[i], conv_b[i], conv_ln_g[i], conv_ln_b[i], w_conv_out[i])
        g = jax.nn.sigmoid(gates + b_gate[i]).reshape(B, S, N_BRANCH, D)
        mixed = g[:, :, 0, :] * y_att + g[:, :, 1, :] * y_conv
        x = x + mixed @ w_out[i]
        h2 = rms_norm(x, ffn_norm_g[i])
        x = x + peer(h2, w_peer_q[i], peer_sub_keys[i], peer_u[i], peer_v[i])
        h3 = rms_norm(x, ple_norm_g[i])
        x = x + jax.nn.sigmoid(h3 @ w_ple_gate[i]) * (p[i] @ w_ple_proj[i])
    return x
```

```python
import math
from contextlib import ExitStack
import numpy as np
import concourse.bass as bass
import concourse.mybir as mybir
from concourse.bass_utils import run_bass_kernel_spmd

F32 = mybir.dt.float32
BF16 = mybir.dt.bfloat16
I32 = mybir.dt.int32
U32 = mybir.dt.uint32
AF = mybir.ActivationFunctionType
ALU = mybir.AluOpType
AX = mybir.AxisListType

D = 1024
S = 8192
NT = 4096
NJ = 32
EPS = 1e-6
NEG_SEL = -3.0e38
MASKV = -30000.0


class Buf:
    __slots__ = ("w", "r")

    def __init__(self):
        self.w = None
        self.r = {}


class Eng:
    def __init__(self, name, obj):
        self.name, self.obj = name, obj
        self.sem = None
        self.key = None
        self.count = 0
        self.epoch = -1
        self.seen = {}


class DmaQ:
    def __init__(self, name, host, nslots):
        self.name, self.host, self.nslots = name, host, nslots
        self.n = 0
        self.sems = [None] * nslots
        self.keys = [None] * nslots
        self.gens = [0] * nslots
        self.epochs = [0] * nslots


class FW:
    EPOCH = 20000
    DGEN = 1500

    def __init__(self, nc, es):
        self.nc, self.es = nc, es
        self.nsem = 0
        self.PE = Eng("pe", nc.tensor)
        self.ACT = Eng("act", nc.scalar)
        self.DVE = Eng("dve", nc.vector)
        self.POOL = Eng("pool", nc.gpsimd)
        self.SP = Eng("sp", nc.sync)
        self.engs = [self.PE, self.ACT, self.DVE, self.POOL, self.SP]
        self.QSP = DmaQ("qsp", self.SP, 6)
        self.QPL = DmaQ("qpl", self.POOL, 6)
        self.QAC = DmaQ("qac", self.ACT, 4)
        self.qs = [self.QSP, self.QPL, self.QAC]
        self.last = {}

    def _newsem(self, name):
        self.nsem += 1
        return self.es.enter_context(self.nc.semaphore(f"{name}_{self.nsem}"))

    def _wait(self, E, rec):
        key, sem, val = rec
        if E.seen.get(key, 0) >= val:
            return
        E.obj.wait_ge(sem, val)
        E.seen[key] = val

    def _deps(self, reads, writes):
        deps = []
        for b in reads:
            if b.w is not None:
                deps.append(b.w)
        for b in writes:
            if b.w is not None:
                deps.append(b.w)
            deps.extend(b.r.values())
        return deps

    def _mark(self, rec, reads, writes):
        for b in reads:
            old = b.r.get(rec[0])
            if old is None or old[2] < rec[2]:
                b.r[rec[0]] = rec
        for b in writes:
            b.w = rec
            b.r = {}
        self.last[rec[0]] = rec

    def op(self, E, fn, reads=(), writes=()):
        if E.sem is None or E.count >= self.EPOCH:
            E.epoch += 1
            E.sem = self._newsem(E.name)
            E.key = f"{E.name}{E.epoch}"
            E.count = 0
        for rec in self._deps(reads, writes):
            if E is self.PE and rec[0].startswith("pe"):
                continue
            self._wait(E, rec)
        inst = fn(E.obj)
        E.count += 1
        inst.then_inc(E.sem, 1)
        rec = (E.key, E.sem, E.count)
        self._mark(rec, reads, writes)
        return rec

    def dma(self, Q, fn, reads=(), writes=()):
        H = Q.host
        for rec in self._deps(reads, writes):
            self._wait(H, rec)
        slot = Q.n % Q.nslots
        if Q.sems[slot] is None or Q.gens[slot] >= self.DGEN:
            if Q.sems[slot] is not None:
                self._wait(H, (Q.keys[slot], Q.sems[slot], 16 * Q.gens[slot]))
            Q.epochs[slot] += 1
            Q.sems[slot] = self._newsem(f"{Q.name}{slot}")
            Q.keys[slot] = f"{Q.name}{slot}e{Q.epochs[slot]}"
            Q.gens[slot] = 0
        if Q.gens[slot] > 0:
            self._wait(H, (Q.keys[slot], Q.sems[slot], 16 * Q.gens[slot]))
        inst = fn(H.obj)
        inst.then_inc(Q.sems[slot], 16)
        Q.gens[slot] += 1
        rec = (Q.keys[slot], Q.sems[slot], 16 * Q.gens[slot])
        Q.n += 1
        self._mark(rec, reads, writes)
        return rec

    def barrier(self):
        recs = list(self.last.values())
        for E in self.engs:
            for rec in recs:
                self._wait(E, rec)

    def pe(self, fn, r=(), w=()):
        return self.op(self.PE, fn, r, w)

    def act(self, fn, r=(), w=()):
        return self.op(self.ACT, fn, r, w)

    def dve(self, fn, r=(), w=()):
        return self.op(self.DVE, fn, r, w)

    def pool(self, fn, r=(), w=()):
        return self.op(self.POOL, fn, r, w)

    def dsp(self, fn, r=(), w=()):
        return self.dma(self.QSP, fn, r, w)

    def dpl(self, fn, r=(), w=()):
        return self.dma(self.QPL, fn, r, w)

    def dac(self, fn, r=(), w=()):
        return self.dma(self.QAC, fn, r, w)


def xap(ap, pat):
    return bass.AP(ap.tensor, ap.offset, [list(ap.ap[0])] + [list(p) for p in pat])


def build_program():
    nc = bass.Bass("TRN2", target_bir_lowering=False)

    def din(name, shape, dt=F32):
        return nc.dram_tensor(name, list(shape), dt, kind="ExternalInput").ap()

    xs = din("xs", [S, D]); xq = din("xq", [NT, D]); xh = din("xh", [NJ * 32, D]); pq = din("pq", [NT, 256])
    w_in = din("w_in", [D, 5192])
    relb = din("rel_bias", [32, 8]); oh = din("oh", [32, 1536])
    g_attn = din("g_attn", [128, 8]); g_ffn = din("g_ffn", [128, 8]); g_ple = din("g_ple", [128, 8])
    g_ffn_bc = din("g_ffn_bc", [128, D]); g_attn_bc = din("g_attn_bc", [128, D])
    gq_bc = din("gq_bc", [128, 512]); gk_bc = din("gk_bc", [128, 512])
    bgate = din("bgate", [128, 16])
    w_ao = din("w_att_out", [512, D]); w_co = din("w_conv_out", [512, D]); w_out = din("w_out", [D, D])
    convw = din("convw", [128, 4, 31]); convb = din("convb", [128, 4]); lng = din("lng", [128, 4]); lnb = din("lnb", [128, 4])
    w_pq = din("w_peer_q", [D, 2048]); subk = din("subk", [128, 16, 128])
    peer_u = din("peer_u", [16384, D]); peer_v = din("peer_v", [16384, D])
    w_pg = din("w_ple_gate", [D, D]); w_pp = din("w_ple_proj", [256, D])
    ident_d = din("ident", [128, 128]); anti_d = din("anti", [128, 128]); vm_d = din("vm", [128, 256])
    iota16_d = din("iota16", [128, 16])
    pw2_d = din("pw2", [128, 32])
    out_d = nc.dram_tensor("out", [NT, D], F32, kind="ExternalOutput").ap()
    kTs_t = nc.dram_tensor("kTs", [512, S], BF16, kind="Internal"); kTs = kTs_t.ap()
    vs_t = nc.dram_tensor("vs", [S, 520], BF16, kind="Internal"); vs = vs_t.ap()
    gtab_t = nc.dram_tensor("gtab", [8, 1536], BF16, kind="Internal"); gtab = gtab_t.ap()
    x1s_t = nc.dram_tensor("x1s", [NT, D], F32, kind="Internal"); x1s = x1s_t.ap()
    attTs_t = nc.dram_tensor("attTs", [512, NT], BF16, kind="Internal")
    uv_bf = nc.dram_tensor("uv_bf", [16384, 2 * D], BF16, kind="Internal").ap()
    B_ubf = Buf()

    es_all = ExitStack()
    fw = FW(nc, es_all)

    def wview(w, c0, c1):
        return w[:, c0:c1].rearrange("(c p) n -> p c n", p=128)

    def SB(es, name, shape, dt):
        return es.enter_context(nc.sbuf_tensor(name, list(shape), dt))

    def PS(es, name, shape, dt=F32):
        return es.enter_context(nc.psum_tensor(name, list(shape), dt))

    ident_f = SB(es_all, "ident_f", [128, 128], F32)
    ident_b = SB(es_all, "ident_b", [128, 128], BF16)
    anti_b = SB(es_all, "anti_b", [128, 128], BF16)
    ones_f = SB(es_all, "ones_f", [128, 128], F32)
    B_const = Buf()
    B_attT = [Buf() for _ in range(NJ)]
    fw.dsp(lambda e: e.dma_start(out=ident_f[:], in_=ident_d), w=[B_const])
    fw.dpl(lambda e: e.dma_start(out=ident_b[:], in_=ident_d), w=[B_const])
    fw.dpl(lambda e: e.dma_start(out=anti_b[:], in_=anti_d), w=[B_const])
    fw.pool(lambda e: e.memset(ones_f[:], 1.0), w=[B_const])

    def rms_hT(es_tmp, tag):
        t = {}
        t["ssq"] = SB(es_tmp, f"rs_{tag}", [128, 4], F32)
        t["xn"] = SB(es_tmp, f"rx_{tag}", [128, D], BF16)
        t["B"] = Buf()
        t["Bch"] = Buf()
        return t

    def emit_rms(t, xt, Bx, n, g_sb, Bg, psT, BpsT, hT_ap_fn, BhT, gbc=None, h_f32=None, Bh32=None, hT_all=None, phase="ab"):
        fused = gbc is not None and hT_all is not None
        if "a" in phase:
            fw.act(lambda e: e.activation(out=t["xn"][0:n, :], in_=xt, func=AF.Square, accum_out=t["ssq"][0:n, 0:1]), r=[Bx], w=[t["B"]])
            fw.act(lambda e: e.activation(out=t["ssq"][0:n, 1:2], in_=t["ssq"][0:n, 0:1], func=AF.Sqrt, scale=1.0 / D, bias=EPS),
                   r=[t["B"]], w=[t["B"]])
            fw.dve(lambda e: e.reciprocal(out=t["ssq"][0:n, 2:3], in_=t["ssq"][0:n, 1:2]), r=[t["B"]], w=[t["B"]])
            if fused:
                fw.dve(lambda e: e.scalar_tensor_tensor(out=t["xn"][0:n, :], in0=xt, scalar=t["ssq"][0:n, 2:3], in1=gbc[0:n, :],
                                                        op0=ALU.mult, op1=ALU.mult), r=[Bx, t["B"], Bg], w=[t["B"]])
            else:
                fw.dve(lambda e: e.tensor_scalar(out=t["xn"][0:n, :], in0=xt, scalar1=t["ssq"][0:n, 2:3], scalar2=None, op0=ALU.mult),
                       r=[Bx, t["B"]], w=[t["B"]])
            if h_f32 is not None:
                fw.dve(lambda e: e.scalar_tensor_tensor(out=h_f32, in0=xt, scalar=t["ssq"][0:n, 2:3], in1=gbc,
                                                        op0=ALU.mult, op1=ALU.mult), r=[Bx, t["B"], Bg], w=[Bh32])
        if "b" not in phase:
            return
        for c in range(8):
            fw.pe(lambda e, c=c: e.transpose(out=psT[:, c, 0:n], in_=t["xn"][0:n, c * 128:(c + 1) * 128], identity=ident_b[0:n, 0:n]),
                  r=[t["B"], B_const], w=[BpsT])
        if fused:
            fw.act(lambda e: e.activation(out=hT_all, in_=psT[:, :, 0:n], func=AF.Copy), r=[BpsT], w=list(BhT))
        else:
            for c in range(8):
                if c % 2 == 0:
                    fw.act(lambda e, c=c: e.activation(out=hT_ap_fn(c), in_=psT[:, c, 0:n], func=AF.Copy, scale=g_sb[:, c:c + 1]),
                           r=[BpsT, Bg], w=[BhT[c], t["Bch"]])
                else:
                    fw.dve(lambda e, c=c: e.tensor_scalar(out=hT_ap_fn(c), in0=psT[:, c, 0:n], scalar1=g_sb[:, c:c + 1], scalar2=None, op0=ALU.mult),
                           r=[BpsT, Bg], w=[BhT[c], t["Bch"]])

    def emit_qknorm(tq, ps_ap, Bps, gbc, Bg, scale, out_bf, Bout):
        fw.act(lambda e: e.activation(func=AF.Copy, out=tq["f"][:], in_=ps_ap), r=[Bps], w=[tq["B"]])
        fw.dve(lambda e: e.tensor_tensor(out=tq["sq"][:], in0=tq["f"][:], in1=tq["f"][:], op=ALU.mult), r=[tq["B"]], w=[tq["B2"]])
        fw.dve(lambda e: e.tensor_reduce(out=tq["s8"][:, 0:8], in_=xap(tq["sq"][:], [[64, 8], [1, 64]]), axis=AX.X, op=ALU.add),
               r=[tq["B2"]], w=[tq["B3"]])
        fw.act(lambda e: e.activation(out=tq["s8"][:, 8:16], in_=tq["s8"][:, 0:8], func=AF.Sqrt, scale=1.0 / 64, bias=EPS),
               r=[tq["B3"]], w=[tq["B3"]])
        fw.dve(lambda e: e.reciprocal(out=tq["s8"][:, 16:24], in_=tq["s8"][:, 8:16]), r=[tq["B3"]], w=[tq["B3"]])
        fw.dve(lambda e: e.tensor_tensor(out=xap(tq["sq"][:], [[64, 8], [1, 64]]), in0=xap(tq["f"][:], [[64, 8], [1, 64]]),
                                         in1=xap(tq["s8"][:, 16:24], [[1, 8], [0, 64]]), op=ALU.mult),
               r=[tq["B"], tq["B3"]], w=[tq["B2"]])
        fw.dve(lambda e: e.scalar_tensor_tensor(out=out_bf, in0=tq["sq"][:], scalar=float(scale), in1=gbc, op0=ALU.mult, op1=ALU.mult),
               r=[tq["B2"], Bg], w=[Bout])

    def mk_qk(es, tag):
        return {"f": SB(es, f"qf_{tag}", [128, 512], F32), "sq": SB(es, f"qs_{tag}", [128, 512], F32),
                "s8": SB(es, f"q8_{tag}", [128, 24], F32), "B": Buf(), "B2": Buf(), "B3": Buf()}

    with ExitStack() as es:
        g_sb = SB(es, "g_attn_sb", [128, 8], F32); Bg = Buf()
        gq_sb = SB(es, "gq_sb", [128, 512], F32); gk_sb = SB(es, "gk_sb", [128, 512], F32)
        vm_sb = SB(es, "vm_sb", [128, 256], F32)
        gabc = SB(es, "gabc", [128, D], F32)
        fw.dsp(lambda e: e.dma_start(out=gabc[:], in_=g_attn_bc), w=[Bg])
        pw_sb = SB(es, "pw_sb", [128, 32], F32)
        cbias = SB(es, "cbias", [128, 8], F32)
        fw.dsp(lambda e: e.dma_start(out=cbias[:], in_=bass.AP(relb.tensor, 15 * 8, [[0, 128], [1, 8]])), w=[Bg])
        fw.dsp(lambda e: e.dma_start(out=pw_sb[:], in_=pw2_d), w=[Bg])
        fw.dsp(lambda e: e.dma_start(out=g_sb[:], in_=g_attn), w=[Bg])
        fw.dsp(lambda e: e.dma_start(out=gq_sb[:], in_=gq_bc), w=[Bg])
        fw.dsp(lambda e: e.dma_start(out=gk_sb[:], in_=gk_bc), w=[Bg])
        fw.dsp(lambda e: e.dma_start(out=vm_sb[:], in_=vm_d), w=[Bg])
        kiT = SB(es, "kiT", [128, S], BF16); B_ki = [Buf() for _ in range(64)]
        TB = SB(es, "TB", [128, 8, 11, 128], BF16); B_TB = Buf()
        with ExitStack() as es2:
            rb_sb = SB(es2, "rb_sb", [32, 8], F32); oh_sb = SB(es2, "oh_sb", [32, 1536], F32)
            g_bf = SB(es2, "g_bf", [8, 1536], BF16)
            psg = PS(es2, "psg", [8, 512]); Bt = Buf(); Bp = Buf(); Bgt = Buf()
            fw.dsp(lambda e: e.dma_start(out=rb_sb[:], in_=relb), w=[Bt])
            fw.dsp(lambda e: e.dma_start(out=oh_sb[:], in_=oh), w=[Bt])
            for i in range(3):
                fw.pe(lambda e, i=i: e.matmul(psg[:], rb_sb[:], oh_sb[:, i * 512:(i + 1) * 512], start=True, stop=True), r=[Bt], w=[Bp])
                fw.act(lambda e, i=i: e.activation(func=AF.Copy, out=g_bf[:, i * 512:(i + 1) * 512], in_=psg[:]), r=[Bp], w=[Bgt])
            fw.dsp(lambda e: e.dma_start(out=gtab, in_=g_bf[:]), r=[Bgt], w=[B_TB])
            for h in range(8):
                for m in range(11):
                    ip = 10 - m
                    src = bass.AP(gtab_t, h * 1536 + 128 * ip, [[1, 128], [1, 128]])
                    fw.dsp(lambda e, h=h, m=m, src=src: e.dma_start(out=TB[:, h, m, :], in_=src), r=[B_TB], w=[Buf()])
            fw.barrier()

        with ExitStack() as es2:
            wk = SB(es2, "wk", [128, 8, 512], BF16); wv = SB(es2, "wv", [128, 8, 512], BF16)
            wki = SB(es2, "wki", [128, 8, 128], BF16); Bw = Buf()
            fw.dpl(lambda e: e.dma_start(out=wk[:], in_=wview(w_in, 512, 1024)), w=[Bw])
            fw.dpl(lambda e: e.dma_start(out=wv[:], in_=wview(w_in, 1024, 1536)), w=[Bw])
            fw.dpl(lambda e: e.dma_start(out=wki[:, :, 0:64], in_=wview(w_in, 2048, 2112)), w=[Bw])
            fw.dpl(lambda e: e.dma_start(out=wki[:, :, 64:128], in_=wview(w_in, 2048, 2112)), w=[Bw])
            for tsrc, c0 in ((peer_u, 0), (peer_v, D)):
                for ch in range(16):
                    fw.dpl(lambda e, tsrc=tsrc, c0=c0, ch=ch: e.dma_start(
                        out=uv_bf[ch * 1024:(ch + 1) * 1024, c0:c0 + D].rearrange("(p r) d -> p r d", p=128),
                        in_=tsrc[ch * 1024:(ch + 1) * 1024, :].rearrange("(p r) d -> p r d", p=128)), w=[B_ubf])
            xt = [SB(es2, f"xtA{i}", [128, D], F32) for i in range(3)]; Bxt = [Buf() for _ in range(3)]
            rtA = [rms_hT(es2, "A0"), rms_hT(es2, "A1")]
            hT = [SB(es2, f"hTA{i}", [128, 8, 128], BF16) for i in range(2)]; BhT = [[Buf() for _ in range(8)], [Buf() for _ in range(8)]]
            psT = PS(es2, "psTA", [128, 8, 128], BF16); BpsT = Buf()
            pk = PS(es2, "pkA", [128, 512]); Bpk = Buf()
            pv = PS(es2, "pvA", [128, 512]); Bpv = Buf()
            pki = PS(es2, "pkiA", [128, 128]); Bpki = Buf()
            pkt = PS(es2, "pktA", [128, 4, 128], BF16); Bpkt = Buf()
            tq = mk_qk(es2, "A")
            kn = [SB(es2, f"knA{i}", [128, 512], BF16) for i in range(2)]; Bkn = [Buf(), Buf()]
            kt_sb = [SB(es2, f"ktA{i}", [128, 4, 128], BF16) for i in range(2)]; Bkt = [Buf(), Buf()]
            v_sb = [SB(es2, f"vA{i}", [128, 8, 65], BF16) for i in range(2)]; Bv = [Buf(), Buf()]
            for i in range(2):
                fw.dve(lambda e, i=i: e.memset(v_sb[i][:], 1.0), w=[Bv[i]])
            B_kTs = Buf(); B_vs = Buf()

            def A0(kb):
                fw.dsp(lambda e: e.dma_start(out=xt[kb % 3][:], in_=xs[kb * 128:(kb + 1) * 128, :]), w=[Bxt[kb % 3]])

            def A1a(kb):
                s = kb % 2
                emit_rms(rtA[s], xt[kb % 3][:], Bxt[kb % 3], 128, g_sb, Bg, psT, BpsT, lambda c: hT[s][:, c, :], BhT[s], gbc=gabc[:], hT_all=hT[s][:],
                         phase="a")

            def A1(kb):
                s = kb % 2
                emit_rms(rtA[s], xt[kb % 3][:], Bxt[kb % 3], 128, g_sb, Bg, psT, BpsT, lambda c: hT[s][:, c, :], BhT[s], gbc=gabc[:], hT_all=hT[s][:],
                         phase="b")

            def A2a(kb):
                s = kb % 2
                for c in range(8):
                    fw.pe(lambda e, c=c: e.matmul(pk[:], hT[s][:, c, :], wk[:, c, :], start=(c == 0), stop=(c == 7)), r=[BhT[s][c], Bw], w=[Bpk])
                for c in range(8):
                    fw.pe(lambda e, c=c: e.matmul(pv[:], hT[s][:, c, :], wv[:, c, :], start=(c == 0), stop=(c == 7)), r=[BhT[s][c], Bw], w=[Bpv])
                for c in range(8):
                    fw.pe(lambda e, c=c: e.matmul(pki[:], wki[:, c, :], hT[s][:, c, :], start=(c == 0), stop=(c == 7)), r=[BhT[s][c], Bw], w=[Bpki])
                fw.act(lambda e: e.activation(func=AF.Copy, out=kiT[:, kb * 128:(kb + 1) * 128], in_=pki[:]), r=[Bpki], w=[B_ki[kb]])
                fw.act(lambda e: e.activation(func=AF.Copy, out=v_sb[s][:, :, 0:64], in_=xap(pv[:], [[64, 8], [1, 64]])), r=[Bpv], w=[Bv[s]])
                fw.dac(lambda e: e.dma_start(out=vs[kb * 128:(kb + 1) * 128, :], in_=xap(v_sb[s][:], [[1, 520]])), r=[Bv[s]], w=[Buf()])
                emit_qknorm(tq, pk[:], Bpk, gk_sb[:], Bg, 1.0, kn[s][:], Bkn[s])

            def A2b(kb):
                s = kb % 2
                for pr in range(4):
                    fw.pe(lambda e, pr=pr: e.transpose(out=pkt[:, pr, :], in_=kn[s][:, pr * 128:(pr + 1) * 128], identity=ident_b[:]),
                          r=[Bkn[s], B_const], w=[Bpkt])
                fw.act(lambda e: e.activation(func=AF.Copy, out=kt_sb[s][:], in_=pkt[:]), r=[Bpkt], w=[Bkt[s]])
                dst = bass.AP(kTs_t, kb * 128, [[S, 128], [128 * S, 4], [1, 128]])
                fw.dac(lambda e: e.dma_start(out=dst, in_=kt_sb[s][:]), r=[Bkt[s]], w=[Buf()])

            A0(0); A0(1)
            A1a(0); A1(0); A1a(1)
            for kb in range(64):
                if kb + 2 < 64:
                    A0(kb + 2)
                if kb + 1 < 64:
                    A1(kb + 1)
                if kb + 2 < 64:
                    A1a(kb + 2)
                A2a(kb)
                if kb >= 1:
                    A2b(kb - 1)
            A2b(63)
            fw.barrier()

        with ExitStack() as es2:
            wq = SB(es2, "wq", [128, 8, 512], BF16); wqi = SB(es2, "wqi", [128, 8, 512], BF16)
            wwi = SB(es2, "wwi", [128, 8, 8], BF16); Bw = Buf()
            fw.dpl(lambda e: e.dma_start(out=wq[:], in_=wview(w_in, 0, 512)), w=[Bw])
            fw.dpl(lambda e: e.dma_start(out=wqi[:], in_=wview(w_in, 1536, 2048)), w=[Bw])
            fw.dpl(lambda e: e.dma_start(out=wwi[:], in_=wview(w_in, 2112, 2120)), w=[Bw])
            xt = [SB(es2, f"xtB{i}", [128, D], F32) for i in range(2)]; Bxt = [Buf(), Buf()]
            rtB = [rms_hT(es2, "B0"), rms_hT(es2, "B1")]
            hT = SB(es2, "hTB", [128, 8, 128], BF16); BhT = [Buf() for _ in range(8)]
            tq = mk_qk(es2, "B")
            qn = SB(es2, "qnB", [128, 512], BF16); Bqn = Buf()
            qib = SB(es2, "qib", [128, 512], BF16); Bqib = Buf()
            qT = [SB(es2, f"qT{i}", [128, 4, 128], BF16) for i in range(2)]; BqT = [Buf(), Buf()]
            qiT = SB(es2, "qiT", [128, 4, 128], BF16); BqiT = Buf()
            wi_sb = SB(es2, "wi_sb", [128, 8], F32); Bwi = Buf()
            NRL = 4
            relu_sb = [SB(es2, f"relu{i}", [128, 512], BF16) for i in range(NRL)]; Brelu = [Buf() for _ in range(NRL)]
            scores = SB(es2, "scores", [128, S], F32); Bsc = Buf()
            tk = SB(es2, "tk", [128, 8], F32); Btk = Buf(); Blo = Buf(); Bw0 = Buf(); Bmid = Buf(); Bcnt = Buf(); Bc = Buf()
            am = SB(es2, "am", [128, 16], F32); Bam = Buf()
            wt = SB(es2, "wt", [128, 32], F32); Bwt = Buf()
            cu = SB(es2, "cu", [128, 2], U32)
            madd = [SB(es2, f"madd{i}", [128, S], BF16) for i in range(2)]; Bmadd = [Buf(), Buf()]
            NSL = 2
            kTc = [SB(es2, f"kTc{i}", [128, 4, 512], BF16) for i in range(NSL)]; BkTc = [Buf() for _ in range(NSL)]
            vc = [SB(es2, f"vc{i}", [128, 4, 520], BF16) for i in range(NSL)]; Bvc = [Buf() for _ in range(NSL)]
            PT = [SB(es2, f"PT{i}", [128, 4, 128], BF16) for i in range(3)]; BPT = [Buf() for _ in range(3)]
            rden = SB(es2, "rden", [128, 8], F32); Brd = Buf()
            att = SB(es2, "att", [128, 512], BF16); Batt = Buf()
            attTb = SB(es2, "attTb", [128, 4, 128], BF16); BattTb = Buf()
            pTP = PS(es2, "pTP", [128, 8, 128], BF16); BpTP = Buf()
            pD = [PS(es2, f"pD{i}", [128, 512]) for i in range(2)]; BpD = [Buf(), Buf()]
            pS = PS(es2, "pS", [128, 512]); BpS = Buf()
            pP = pS; BpP = BpS
            pST = [PS(es2, f"pST{i}", [128, 4, 128]) for i in range(2)]; BpST = [Buf(), Buf()]
            pO = [PS(es2, f"pO{i}", [128, 4, 65]) for i in range(2)]; BpO = [Buf(), Buf()]

            def prologue_a(j):
                s = j % 2
                fw.dsp(lambda e: e.dma_start(out=xt[s][:], in_=xq[j * 128:(j + 1) * 128, :]), w=[Bxt[s]])
                emit_rms(rtB[s], xt[s][:], Bxt[s], 128, g_sb, Bg, pTP, BpTP, lambda c: hT[:, c, :], BhT, gbc=gabc[:], hT_all=hT[:], phase="a")

            def prologue(j):
                s = j % 2
                emit_rms(rtB[s], xt[s][:], Bxt[s], 128, g_sb, Bg, pTP, BpTP, lambda c: hT[:, c, :], BhT, gbc=gabc[:], hT_all=hT[:], phase="b")
                for c in range(8):
                    fw.pe(lambda e, c=c: e.matmul(pP[:], hT[:, c, :], wq[:, c, :], start=(c == 0), stop=(c == 7)), r=[BhT[c], Bw], w=[BpP])
                emit_qknorm(tq, pP[:], BpP, gq_sb[:], Bg, 0.125, qn[:], Bqn)
                for c in range(8):
                    fw.pe(lambda e, c=c: e.matmul(pP[:], hT[:, c, :], wqi[:, c, :], start=(c == 0), stop=(c == 7)), r=[BhT[c], Bw], w=[BpP])
                fw.act(lambda e: e.activation(func=AF.Copy, out=qib[:], in_=pP[:]), r=[BpP], w=[Bqib])
                for pr in range(4):
                    fw.pe(lambda e, pr=pr: e.transpose(out=pTP[:, pr, :], in_=qib[:, pr * 128:(pr + 1) * 128], identity=ident_b[:]),
                          r=[Bqib, B_const], w=[BpTP])
                fw.act(lambda e: e.activation(func=AF.Copy, out=qiT[:], in_=pTP[:, 0:4, :]), r=[BpTP], w=[BqiT])
                for c in range(8):
                    fw.pe(lambda e, c=c: e.matmul(pP[:, 0:8], hT[:, c, :], wwi[:, c, :], start=(c == 0), stop=(c == 7)), r=[BhT[c], Bw], w=[BpP])
                fw.act(lambda e: e.activation(func=AF.Copy, out=wi_sb[:], in_=pP[:, 0:8]), r=[BpP], w=[Bwi])

            def prologue_q(j):
                s = j % 2
                for pr in range(4):
                    fw.pe(lambda e, pr=pr: e.transpose(out=pTP[:, pr, :], in_=qn[:, pr * 128:(pr + 1) * 128], identity=ident_b[:]),
                          r=[Bqn, B_const], w=[BpTP])
                fw.act(lambda e: e.activation(func=AF.Copy, out=qT[s][:], in_=pTP[:, 0:4, :]), r=[BpTP], w=[BqT[s]])

            cnt = {"relu": 0, "d": 0}

            def indexer(j):
                NK = (2 * j + 2) * 128
                nkt = (NK + 511) // 512
                for kt in range(nkt):
                    k0 = kt * 512
                    w = min(512, NK - k0)
                    kbs = [B_ki[b] for b in range(k0 // 128, (k0 + w) // 128)]
                    for h in range(8):
                        d = cnt["d"] % 2; cnt["d"] += 1
                        hp = h % 2
                        fw.pe(lambda e: e.matmul(pD[d][:, 0:w], qiT[hp * 64:(hp + 1) * 64, h // 2, :], kiT[hp * 64:(hp + 1) * 64, k0:k0 + w],
                                                 start=True, stop=True), r=[BqiT] + kbs, w=[BpD[d]])
                        rs = cnt["relu"] % NRL; cnt["relu"] += 1
                        fw.act(lambda e: e.activation(out=relu_sb[rs][:, 0:w], in_=pD[d][:, 0:w], func=AF.Relu), r=[BpD[d]], w=[Brelu[rs]])
                        if h == 0:
                            fw.act(lambda e: e.activation(out=scores[:, k0:k0 + w], in_=relu_sb[rs][:, 0:w], func=AF.Copy, scale=wi_sb[:, 0:1]),
                                   r=[Brelu[rs], Bwi], w=[Bsc])
                        else:
                            fw.dve(lambda e: e.scalar_tensor_tensor(out=scores[:, k0:k0 + w], in0=relu_sb[rs][:, 0:w], scalar=wi_sb[:, h:h + 1],
                                                                    in1=scores[:, k0:k0 + w], op0=ALU.mult, op1=ALU.add),
                                   r=[Brelu[rs], Bwi, Bsc], w=[Bsc])
                fw.dve(lambda e: e.tensor_tensor(out=scores[:, NK - 256:NK], in0=scores[:, NK - 256:NK], in1=vm_sb[:], op=ALU.add),
                       r=[Bsc, Bg], w=[Bsc])

            NIT = 24

            def topk(j):
                NK = (2 * j + 2) * 128
                nkt = (NK + 511) // 512
                ms = j % 2
                if j == 0:
                    fw.dve(lambda e: e.tensor_scalar(out=madd[ms][:, 0:NK], in0=scores[:, 0:NK], scalar1=-1.0e8, scalar2=MASKV,
                                                     op0=ALU.is_le, op1=ALU.mult), r=[Bsc], w=[Bmadd[ms]])
                    return
                fw.dve(lambda e: e.tensor_reduce(out=tk[:, 0:1], in_=scores[:, 0:NK - 256], axis=AX.X, op=ALU.max, apply_absolute_value=True),
                       r=[Bsc], w=[Btk])
                fw.dve(lambda e: e.tensor_scalar(out=tk[:, 2:3], in0=tk[:, 0:1], scalar1=2.002, scalar2=2.0e-6, op0=ALU.mult, op1=ALU.add), r=[Btk], w=[Bw0])
                fw.dve(lambda e: e.tensor_scalar(out=wt[:], in0=pw_sb[:], scalar1=tk[:, 2:3], scalar2=None, op0=ALU.mult), r=[Bw0, Bg], w=[Bwt])
                fw.dve(lambda e: e.tensor_scalar(out=tk[:, 1:2], in0=tk[:, 0:1], scalar1=-1.001, scalar2=-1.0e-6, op0=ALU.mult, op1=ALU.add), r=[Btk], w=[Blo])
                Bj = Buf()
                for it in range(NIT):
                    fw.dve(lambda e, it=it: e.tensor_tensor(out=tk[:, 3:4], in0=tk[:, 1:2], in1=wt[:, it:it + 1], op=ALU.add), r=[Blo, Bwt], w=[Bmid])
                    fw.dve(lambda e: e.tensor_scalar(out=madd[ms][:, 0:NK], in0=scores[:, 0:NK], scalar1=tk[:, 3:4], scalar2=0.0,
                                                     op0=ALU.is_ge, op1=ALU.add, accum_out=tk[:, 4:5]),
                           r=[Bsc, Bmid], w=([Bmadd[ms], Bj, Bcnt] if it == 0 else [Bj, Bcnt]))
                    fw.dve(lambda e: e.memset(tk[:, 6:7], 0.0), w=[Bcnt])
                    fw.dve(lambda e: e.tensor_scalar(out=cu[:, 0:1], in0=tk[:, 4:5], scalar1=255.5, scalar2=None, op0=ALU.is_ge), r=[Bcnt], w=[Bc])
                    fw.dve(lambda e: e.copy_predicated(out=tk[:, 1:2], mask=cu[:, 0:1], data=tk[:, 3:4]), r=[Bc, Bmid], w=[Blo])
                fw.dve(lambda e: e.tensor_scalar(out=madd[ms][:, 0:NK], in0=scores[:, 0:NK], scalar1=tk[:, 1:2], scalar2=MASKV,
                                                 op0=ALU.is_lt, op1=ALU.mult), r=[Bsc, Blo], w=[Bmadd[ms], Bj])

            acnt = {"g": 0, "st": 0, "pt": 0}

            def attention(j):
                NB = 2 * j + 2
                NG = (NB + 3) // 4
                s = j % 2
                ms = j % 2
                for i in range(2):
                    fw.dve(lambda e, i=i: e.memset(pO[i][:], 0.0), w=[BpO[i]])
                steps = [(g, h) for g in range(NG) for h in range(8)]
                ginfo = {}
                sinfo = {}

                def load_group(g):
                    nb = min(4, NB - 4 * g)
                    sl = acnt["g"] % NSL; acnt["g"] += 1
                    k0 = g * 512
                    srck = bass.AP(kTs_t, k0, [[S, 128], [128 * S, 4], [1, nb * 128]])
                    fw.dsp(lambda e: e.dma_start(out=kTc[sl][:, :, 0:nb * 128], in_=srck), r=[B_kTs], w=[BkTc[sl]])
                    srcv = bass.AP(vs_t, k0 * 520, [[520, 128], [128 * 520, nb], [1, 520]])
                    fw.dsp(lambda e: e.dma_start(out=vc[sl][:, 0:nb, :], in_=srcv), r=[B_vs], w=[Bvc[sl]])
                    ip0 = (2 * j + 1) - 4 * g
                    ginfo[g] = (nb, sl, ip0, max(0, 10 - ip0))

                def S_step(k):
                    g, h = steps[k]
                    if h == 0:
                        load_group(g)
                    nb, sl, ip0, m0 = ginfo[g]
                    st = acnt["st"] % 2; acnt["st"] += 1
                    pt = acnt["pt"] % 3; acnt["pt"] += 1
                    sinfo[k] = pt
                    hp = h % 2
                    all_far = (ip0 - (nb - 1)) >= 7
                    if not all_far:
                        fw.pe(lambda e: e.matmul(pST[st][:, 0:nb, :], anti_b[:], TB[:, h, m0:m0 + nb, :], start=True, stop=False,
                                                 skip_group_check=True), r=[B_const, B_TB], w=[BpST[st]])
                    for b in range(nb):
                        fw.pe(lambda e, b=b: e.matmul(pST[st][:, b, :], kTc[sl][hp * 64:(hp + 1) * 64, h // 2, b * 128:(b + 1) * 128],
                                                      qT[s][hp * 64:(hp + 1) * 64, h // 2, :], start=(all_far and b == 0), stop=False,
                                                      skip_group_check=True),
                              r=[BkTc[sl], BqT[s]], w=[BpST[st]])
                        fw.pe(lambda e, b=b: e.matmul(pST[st][:, b, :], madd[ms][:, (4 * g + b) * 128:(4 * g + b + 1) * 128], ident_b[:],
                                                      start=False, stop=(b == nb - 1), skip_group_check=True),
                              r=[Bmadd[ms], B_const], w=[BpST[st]])
                    if all_far:
                        fw.act(lambda e: e.activation(out=PT[pt][:, 0:nb, :], in_=pST[st][:, 0:nb, :], func=AF.Exp, bias=cbias[:, h:h + 1]),
                               r=[BpST[st], Bg], w=[BPT[pt]])
                    else:
                        fw.act(lambda e: e.activation(out=PT[pt][:, 0:nb, :], in_=pST[st][:, 0:nb, :], func=AF.Exp), r=[BpST[st]], w=[BPT[pt]])

                def PV_step(k):
                    g, h = steps[k]
                    nb, sl, ip0, m0 = ginfo[g]
                    pt = sinfo[k]
                    for b in range(nb):
                        fw.pe(lambda e, b=b: e.matmul(pO[h // 4][:, h % 4, :], PT[pt][:, b, :], vc[sl][:, b, h * 65:(h + 1) * 65], start=False, stop=False,
                                                      skip_group_check=True), r=[BPT[pt], Bvc[sl]], w=[BpO[h // 4]])

                S_step(0)
                for k in range(len(steps)):
                    if k + 1 < len(steps):
                        S_step(k + 1)
                    PV_step(k)

            def attention_epilogue(j):
                for i in range(2):
                    fw.dve(lambda e, i=i: e.reciprocal(out=rden[:, 4 * i:4 * i + 4], in_=xap(pO[i][:, 0, 64:65], [[65, 4]])), r=[BpO[i]], w=[Brd])
                for i in range(2):
                    fw.dve(lambda e, i=i: e.tensor_tensor(out=xap(att[:, 256 * i:256 * i + 256], [[64, 4], [1, 64]]), in0=pO[i][:, :, 0:64],
                                                          in1=xap(rden[:, 4 * i:4 * i + 4], [[1, 4], [0, 64]]), op=ALU.mult),
                           r=[BpO[i], Brd], w=[Batt])
                for pr in range(4):
                    fw.pe(lambda e, pr=pr: e.transpose(out=pTP[:, 4 + pr, :], in_=att[:, pr * 128:(pr + 1) * 128], identity=ident_b[:]),
                          r=[Batt, B_const], w=[BpTP])
                fw.act(lambda e: e.activation(func=AF.Copy, out=attTb[:], in_=pTP[:, 4:8, :]), r=[BpTP], w=[BattTb])
                dsta = bass.AP(attTs_t, j * 128, [[NT, 128], [128 * NT, 4], [1, 128]])
                fw.dac(lambda e: e.dma_start(out=dsta, in_=attTb[:]), r=[BattTb], w=[B_attT[j]])

            prologue_a(0); prologue(0); indexer(0); prologue_q(0); topk(0)
            prologue_a(1)
            for j in range(NJ):
                if j + 1 < NJ:
                    prologue(j + 1); indexer(j + 1); prologue_q(j + 1)
                if j >= 1:
                    attention_epilogue(j - 1)
                if j + 2 < NJ:
                    prologue_a(j + 2)
                attention(j)
                if j + 1 < NJ:
                    topk(j + 1)
            attention_epilogue(NJ - 1)
            fw.barrier()

    with ExitStack() as es:
        g_sb = SB(es, "g_attn_sb2", [128, 8], F32); Bg = Buf()
        bg_sb = SB(es, "bg_sb", [128, 16], F32)
        gabc2 = SB(es, "gabc2", [128, D], F32)
        fw.dsp(lambda e: e.dma_start(out=gabc2[:], in_=g_attn_bc), w=[Bg])
        cw = SB(es, "cw", [128, 4, 31], F32); cb = SB(es, "cb", [128, 4], F32)
        lg = SB(es, "lg", [128, 4], F32); lb = SB(es, "lb", [128, 4], F32)
        for dst_, src_ in ((g_sb, g_attn), (bg_sb, bgate), (cw, convw), (cb, convb), (lg, lng), (lb, lnb)):
            fw.dsp(lambda e, dst_=dst_, src_=src_: e.dma_start(out=dst_[:], in_=src_), w=[Bg])
        wglu = SB(es, "wglu", [128, 8, 1024], BF16); wgate = SB(es, "wgate", [128, 8, 2048], BF16)
        wao = SB(es, "wao", [128, 4, 1024], BF16); wco = SB(es, "wco", [128, 4, 1024], BF16)
        wout = SB(es, "wout", [128, 8, 1024], BF16); Bw = Buf()
        fw.dpl(lambda e: e.dma_start(out=wglu[:], in_=wview(w_in, 2120, 3144)), w=[Bw])
        fw.dpl(lambda e: e.dma_start(out=wgate[:, :, 0:1024], in_=wview(w_in, 3144, 4168)), w=[Bw])
        fw.dpl(lambda e: e.dma_start(out=wgate[:, :, 1024:2048], in_=wview(w_in, 4168, 5192)), w=[Bw])
        fw.dpl(lambda e: e.dma_start(out=wao[:], in_=wview(w_ao, 0, 1024)), w=[Bw])
        fw.dpl(lambda e: e.dma_start(out=wco[:], in_=wview(w_co, 0, 1024)), w=[Bw])
        fw.dpl(lambda e: e.dma_start(out=wout[:], in_=wview(w_out, 0, 1024)), w=[Bw])
        xt = [SB(es, f"xt2{i}", [128, D], F32) for i in range(3)]; Bxt = [Buf() for _ in range(3)]
        xht = [SB(es, f"xh2{i}", [32, D], F32) for i in range(3)]; Bxh = [Buf() for _ in range(3)]
        rt = rms_hT(es, "C"); rt2 = rms_hT(es, "Ch")
        hT = [SB(es, f"hT2{i}", [128, 8, 160], BF16) for i in range(3)]; BhT = [[Buf() for _ in range(8)] for _ in range(3)]; BhTh = [[Buf() for _ in range(8)] for _ in range(3)]
        sig = SB(es, "sig2", [128, 160], F32); Bsig = Buf()
        u = SB(es, "u2", [128, 4, 160], BF16); Bu = [Buf() for _ in range(4)]
        dgw = SB(es, "dgw", [128, 4, 31, 128], BF16); Bdgw = Buf()
        for cc in range(4):
            for k in range(31):
                fw.act(lambda e, cc=cc, k=k: e.activation(out=dgw[:, cc, k, :], in_=ident_b[:], func=AF.Copy, scale=cw[:, cc, k:k + 1]),
                       r=[Bg, B_const], w=[Bdgw])
        pC = PS(es, "pC2", [128, 4, 128]); BpC = Buf()
        yb = SB(es, "yb2", [128, 4, 128], F32); Byb = Buf()
        ysq = SB(es, "ysq2", [128, 4, 128], F32); Bysq = Buf()
        stat = SB(es, "stat2", [128, 4, 128], F32); Bstat = Buf()
        z = SB(es, "z2", [128, 4, 128], F32); Bz = Buf()
        sT = [SB(es, f"sT2{i}", [128, 4, 128], BF16) for i in range(2)]; BsT = [Buf(), Buf()]
        sg = SB(es, "sg2", [128, 2, 128], F32); Bsg = Buf()
        mm_ = SB(es, "mm2", [128, 2, 128], F32); Bmm = Buf()
        mixT = SB(es, "mixT", [128, 8, 128], BF16); BmixT = Buf()
        zer = SB(es, "zer2", [128, 512], F32)
        fw.pool(lambda e: e.memset(zer[:], 0.0), w=[Bg])
        x1t = [SB(es, f"x1t{i}", [128, D], F32) for i in range(2)]; Bx1 = [Buf(), Buf()]
        attT_t = [SB(es, f"attTt{i}", [128, 4, 128], BF16) for i in range(2)]; BattT_t = [Buf(), Buf()]
        pTP = PS(es, "pTP2", [128, 8, 128], BF16); BpTP = Buf()
        pG = PS(es, "pG2", [128, 2, 160]); BpG = Buf()
        pSum = PS(es, "pSum2", [128, 2, 128]); BpSum = Buf()
        pM = [PS(es, f"pM2{i}", [128, 4, 128]) for i in range(2)]; BpM = [Buf(), Buf()]
        pX = [PS(es, f"pX2{i}", [128, 512]) for i in range(2)]; BpX = [Buf(), Buf()]
        B_x1s = Buf()
        mc = {"m": 0}

        def front_a1(j):
            s3 = j % 3
            fw.dsp(lambda e: e.dma_start(out=xt[s3][:], in_=xq[j * 128:(j + 1) * 128, :]), w=[Bxt[s3]])
            fw.dsp(lambda e: e.dma_start(out=xht[s3][:], in_=xh[j * 32:(j + 1) * 32, :]), w=[Bxh[s3]])
            emit_rms(rt2, xht[s3][:], Bxh[s3], 32, g_sb, Bg, pTP, BpTP, lambda c: hT[s3][:, c, 0:32], BhTh[s3], gbc=gabc2[:], hT_all=hT[s3][:, :, 0:32])
            emit_rms(rt, xt[s3][:], Bxt[s3], 128, g_sb, Bg, pTP, BpTP, lambda c: hT[s3][:, c, 32:160], BhT[s3], gbc=gabc2[:], hT_all=hT[s3][:, :, 32:160])

        def front_a2(j):
            s = j % 2
            s3 = j % 3
            srca = bass.AP(attTs_t, j * 128, [[NT, 128], [128 * NT, 4], [1, 128]])
            fw.dsp(lambda e: e.dma_start(out=attT_t[s][:], in_=srca), r=[B_attT[j]], w=[BattT_t[s]])
            for cc in range(4):
                for half in range(2):
                    fc = cc + 4 * half
                    for c in range(8):
                        fw.pe(lambda e, c=c, fc=fc, half=half: e.matmul(pG[:, half, :], wglu[:, c, fc * 128:(fc + 1) * 128], hT[s3][:, c, :],
                                                                        start=(c == 0 and half == 0), stop=(c == 7 and half == 1),
                                                                        skip_group_check=True), r=[BhT[s3][c], BhTh[s3][c], Bw], w=[BpG])
                fw.act(lambda e: e.activation(out=sig[:], in_=pG[:, 1, :], func=AF.Sigmoid), r=[BpG], w=[Bsig])
                fw.dve(lambda e, cc=cc: e.tensor_tensor(out=u[:, cc, :], in0=pG[:, 0, :], in1=sig[:], op=ALU.mult), r=[BpG, Bsig], w=[Bu[cc]])

        def conv_gen(j):
            ops = [(cc, k) for cc in range(4) for k in range(31)]
            per = (len(ops) + 7) // 8
            for chunk in range(8):
                for cc, k in ops[chunk * per:(chunk + 1) * per]:
                    fw.pe(lambda e, cc=cc, k=k: e.matmul(pC[:, cc, :], dgw[:, cc, k, :], u[:, cc, 2 + k:130 + k], start=(k == 0), stop=(k == 30),
                                                         skip_group_check=True), r=[Bu[cc], Bdgw], w=[BpC])
                yield

        def front_c(j):
            s = j % 2
            for cc in range(4):
                fw.act(lambda e, cc=cc: e.activation(out=yb[:, cc, :], in_=pC[:, cc, :], func=AF.Identity, bias=cb[:, cc:cc + 1]),
                       r=[BpC, Bg], w=[Byb])
            fw.act(lambda e: e.activation(out=ysq[:], in_=yb[:], func=AF.Square), r=[Byb], w=[Bysq])
            for cc in range(4):
                fw.pe(lambda e, cc=cc: e.matmul(pSum[:, 0, :], ones_f[:], yb[:, cc, :], start=(cc == 0), stop=False, skip_group_check=True),
                      r=[Byb, B_const], w=[BpSum])
            for cc in range(4):
                fw.pe(lambda e, cc=cc: e.matmul(pSum[:, 1, :], ones_f[:], ysq[:, cc, :], start=False, stop=(cc == 3), skip_group_check=True),
                      r=[Bysq, B_const], w=[BpSum])
            fw.dve(lambda e: e.tensor_scalar(out=stat[:, 0, :], in0=pSum[:, 0, :], scalar1=1.0 / 512, scalar2=None, op0=ALU.mult), r=[BpSum], w=[Bstat])
            fw.dve(lambda e: e.tensor_tensor(out=stat[:, 1, :], in0=stat[:, 0, :], in1=stat[:, 0, :], op=ALU.mult), r=[Bstat], w=[Bstat])
            fw.dve(lambda e: e.scalar_tensor_tensor(out=stat[:, 2, :], in0=pSum[:, 1, :], scalar=1.0 / 512, in1=stat[:, 1, :], op0=ALU.mult, op1=ALU.subtract),
                   r=[BpSum, Bstat], w=[Bstat])
            fw.act(lambda e: e.activation(out=stat[:, 2, :], in_=stat[:, 2, :], func=AF.Sqrt, bias=EPS), r=[Bstat], w=[Bstat])
            fw.dve(lambda e: e.reciprocal(out=stat[:, 3, :], in_=stat[:, 2, :]), r=[Bstat], w=[Bstat])
            fw.dve(lambda e: e.tensor_tensor(out=z[:], in0=yb[:], in1=xap(stat[:, 0, :], [[0, 4], [1, 128]]), op=ALU.subtract), r=[Byb, Bstat], w=[Bz])
            fw.dve(lambda e: e.tensor_tensor(out=z[:], in0=z[:], in1=xap(stat[:, 3, :], [[0, 4], [1, 128]]), op=ALU.mult), r=[Bz, Bstat], w=[Bz])
            for cc in range(4):
                fw.act(lambda e, cc=cc: e.activation(out=sT[s][:, cc, :], in_=z[:, cc, :], func=AF.Silu, scale=lg[:, cc:cc + 1], bias=lb[:, cc:cc + 1]),
                       r=[Bz, Bg], w=[BsT[s]])

        def back(j, cgen):
            s = j % 2
            s3 = j % 3
            for fc in range(8):
                if cgen is not None:
                    next(cgen, None)
                pm = mc["m"] % 2; mc["m"] += 1
                fw.act(lambda e, pm=pm: e.activation(out=xap(pM[pm][:], [[1, 512]]), in_=zer[:], func=AF.Copy), r=[Bg], w=[BpM[pm]])
                for gi in range(2):
                    for c in range(8):
                        fw.pe(lambda e, c=c, gi=gi, fc=fc, pm=pm: e.matmul(pM[pm][:, gi, :], wgate[:, c, gi * 1024 + fc * 128:gi * 1024 + (fc + 1) * 128],
                                                                           hT[s3][:, c, 32:160], start=False, stop=False, skip_group_check=True),
                              r=[BhT[s3][c], Bw], w=[BpM[pm]])
                for cc in range(4):
                    fw.pe(lambda e, cc=cc, fc=fc, pm=pm: e.matmul(pM[pm][:, 2, :], wao[:, cc, fc * 128:(fc + 1) * 128], attT_t[s][:, cc, :],
                                                                  start=False, stop=False, skip_group_check=True), r=[BattT_t[s], Bw], w=[BpM[pm]])
                for cc in range(4):
                    fw.pe(lambda e, cc=cc, fc=fc, pm=pm: e.matmul(pM[pm][:, 3, :], wco[:, cc, fc * 128:(fc + 1) * 128], sT[s][:, cc, :],
                                                                  start=False, stop=False, skip_group_check=True), r=[BsT[s], Bw], w=[BpM[pm]])
                for gi in range(2):
                    fw.act(lambda e, gi=gi, fc=fc, pm=pm: e.activation(out=sg[:, gi, :], in_=pM[pm][:, gi, :], func=AF.Sigmoid,
                                                                       bias=bg_sb[:, gi * 8 + fc:gi * 8 + fc + 1]), r=[BpM[pm], Bg], w=[Bsg])
                fw.dve(lambda e, pm=pm: e.tensor_tensor(out=mm_[:], in0=sg[:], in1=pM[pm][:, 2:4, :], op=ALU.mult), r=[Bsg, BpM[pm]], w=[Bmm])
                fw.dve(lambda e, fc=fc: e.tensor_tensor(out=mixT[:, fc, :], in0=mm_[:, 0, :], in1=mm_[:, 1, :], op=ALU.add), r=[Bmm], w=[BmixT])
            if cgen is not None:
                for _ in cgen:
                    pass

        def back_out(j):
            s = j % 2
            s3 = j % 3
            for half in range(2):
                for fc in range(8):
                    fw.pe(lambda e, fc=fc, half=half: e.matmul(pX[half][:], mixT[:, fc, :], wout[:, fc, half * 512:(half + 1) * 512],
                                                               start=(fc == 0), stop=(fc == 7)), r=[BmixT, Bw], w=[BpX[half]])
                fw.dve(lambda e, half=half: e.tensor_tensor(out=x1t[s][:, half * 512:(half + 1) * 512], in0=pX[half][:],
                                                            in1=xt[s3][:, half * 512:(half + 1) * 512], op=ALU.add), r=[BpX[half], Bxt[s3]], w=[Bx1[s]])
            fw.dsp(lambda e: e.dma_start(out=x1s[j * 128:(j + 1) * 128, :], in_=x1t[s][:]), r=[Bx1[s]], w=[B_x1s])

        front_a1(0)
        front_a2(0)
        front_a1(1)
        for _ in conv_gen(0):
            pass
        front_c(0)
        for j in range(NJ):
            if j + 1 < NJ:
                front_a2(j + 1)
                if j + 2 < NJ:
                    front_a1(j + 2)
                back(j, conv_gen(j + 1))
                front_c(j + 1)
                back_out(j)
            else:
                back(j, None)
                back_out(j)
        fw.barrier()

    with ExitStack() as es:
        gf_sb = SB(es, "gf_sb", [128, 8], F32); gp_sb = SB(es, "gp_sb", [128, 8], F32); Bg = Buf()
        gfbc = SB(es, "gfbc", [128, D], F32); io16 = SB(es, "io16", [128, 16], F32)
        for dst_, src_ in ((gf_sb, g_ffn), (gp_sb, g_ple), (gfbc, g_ffn_bc), (io16, iota16_d)):
            fw.dsp(lambda e, dst_=dst_, src_=src_: e.dma_start(out=dst_[:], in_=src_), w=[Bg])
        wpq = SB(es, "wpq", [128, 8, 2048], BF16); sk = SB(es, "sk", [128, 16, 128], BF16)
        wpg = SB(es, "wpg", [128, 8, 1024], BF16); wpp = SB(es, "wpp", [128, 2, 1024], BF16); Bw = Buf()
        fw.dpl(lambda e: e.dma_start(out=wpq[:, :, 0:1024], in_=wview(w_pq, 0, 1024)), w=[Bw])
        fw.dpl(lambda e: e.dma_start(out=wpq[:, :, 1024:2048], in_=wview(w_pq, 1024, 2048)), w=[Bw])
        fw.dpl(lambda e: e.dma_start(out=sk[:], in_=subk), w=[Bw])
        fw.dpl(lambda e: e.dma_start(out=wpg[:], in_=wview(w_pg, 0, 1024)), w=[Bw])
        fw.dpl(lambda e: e.dma_start(out=wpp[:], in_=wview(w_pp, 0, 1024)), w=[Bw])
        x1 = [SB(es, f"x1c{i}", [128, D], F32) for i in range(2)]; Bx1 = [Buf(), Buf()]
        pt_ = [SB(es, f"ptc{i}", [128, 256], F32) for i in range(2)]; Bpt = [Buf(), Buf()]
        rt = rms_hT(es, "P")
        h2T = SB(es, "h2T", [128, 8, 128], BF16); Bh2T = [Buf() for _ in range(8)]
        h2 = [SB(es, f"h2_{i}", [128, D], F32) for i in range(2)]; Bh2 = [Buf(), Buf()]
        q_sb = SB(es, "q_sb", [128, 2048], BF16); Bq = Buf()
        qTp = SB(es, "qTp", [128, 16, 128], BF16); BqT = Buf()
        sc = SB(es, "sc", [128, 16, 128], F32); Bsc = [Buf() for _ in range(16)]
        sv = SB(es, "sv", [128, 16, 16], F32); Bsv = [Buf() for _ in range(16)]
        si = SB(es, "si", [128, 16, 16], U32); Bsi = [Buf() for _ in range(16)]
        sif = SB(es, "sif", [128, 16, 16], F32); Bsif = Buf()
        cand = SB(es, "cand", [128, 8, 256], F32); Bcand = [Buf() for _ in range(8)]
        tops = SB(es, "tops", [128, 8, 16], F32); Btops = [Buf() for _ in range(8)]
        cj = SB(es, "cj", [128, 8, 16], U32); Bcj = [Buf() for _ in range(8)]
        cji = SB(es, "cji", [128, 2, 128], U32); cjf = SB(es, "cjf", [128, 2, 128], F32); Bcjf = Buf()
        ohx = SB(es, "ohx", [128, 128, 16], F32); Boh = Buf()
        abf = SB(es, "abf", [128, 2, 128], F32); Babf = Buf()
        eidx_f = SB(es, "eidx_f", [128, 128], F32); Beif = Buf()
        eidx = [SB(es, f"eidx{i}", [128, 128], I32) for i in range(2)]; Beidx = [Buf(), Buf()]
        ex = SB(es, "ex", [128, 8, 16], F32); zz = SB(es, "zz", [128, 16], F32); Bex = Buf()
        gate = [SB(es, f"gate{i}", [128, 128], F32) for i in range(2)]; Bgate = [Buf(), Buf()]
        dots = SB(es, "dots", [128, 128], F32); Bdots = [Buf() for _ in range(128)]
        tmpa = SB(es, "tmpa", [128, 128], F32); tmpb = SB(es, "tmpb", [128, 128], F32); Btmp = [Buf() for _ in range(32)]
        actv = SB(es, "actv", [128, 128], F32); Bactv = [Buf() for _ in range(32)]
        GS = 4
        NGB = 16
        gb = [SB(es, f"gb{i}", [128, 2 * D], BF16) for i in range(NGB)]; Bgb = [Buf() for _ in range(NGB)]
        NDG = 4
        dg = [SB(es, f"dg{i}", [128, 128], BF16) for i in range(NDG)]; Bdg = [Buf() for _ in range(NDG)]
        junk = SB(es, "junkp", [128, D], BF16); Bjunk = Buf()
        x2 = SB(es, "x2", [128, D], F32); Bx2 = Buf()
        h3T = SB(es, "h3T", [128, 8, 128], BF16); Bh3T = [Buf() for _ in range(8)]
        pb = SB(es, "pb", [128, 256], BF16); Bpb = Buf()
        pT = SB(es, "pT", [128, 2, 128], BF16); BpT = Buf()
        gsig = SB(es, "gsig", [128, D], F32); Bgsig = Buf()
        pTP = PS(es, "pTP3", [128, 8, 128], BF16); BpTP = Buf()
        pQ = [PS(es, f"pQ3{i}", [128, 512]) for i in range(2)]; BpQ = [Buf() for _ in range(2)]
        pA = [PS(es, f"pA3{i}", [128, 512]) for i in range(2)]; BpA = [Buf(), Buf()]
        pE = [PS(es, f"pE3{i}", [128, 512]) for i in range(2)]; BpE = [Buf(), Buf()]
        B_out = Buf()
        cn = {"g": 0, "d": 0}

        def route(j):
            s = j % 2
            fw.dsp(lambda e: e.dma_start(out=x1[s][:], in_=x1s[j * 128:(j + 1) * 128, :]), r=[B_x1s], w=[Bx1[s]])
            fw.dsp(lambda e: e.dma_start(out=pt_[s][:], in_=pq[j * 128:(j + 1) * 128, :]), w=[Bpt[s]])
            emit_rms(rt, x1[s][:], Bx1[s], 128, gf_sb, Bg, pTP, BpTP, lambda c: h2T[:, c, :], Bh2T, gbc=gfbc[:], h_f32=h2[s][:], Bh32=Bh2[s], hT_all=h2T[:])
            for n in range(4):
                pq_ = n % 2
                for c in range(8):
                    fw.pe(lambda e, c=c, n=n: e.matmul(pQ[pq_][:], h2T[:, c, :], wpq[:, c, n * 512:(n + 1) * 512], start=(c == 0), stop=(c == 7)),
                          r=[Bh2T[c], Bw], w=[BpQ[pq_]])
                fw.act(lambda e, n=n: e.activation(func=AF.Copy, out=q_sb[:, n * 512:(n + 1) * 512], in_=pQ[pq_][:]), r=[BpQ[pq_]], w=[Bq])
            for half in range(2):
                for i in range(8):
                    hc = half * 8 + i
                    fw.pe(lambda e, i=i, hc=hc: e.transpose(out=pTP[:, i, :], in_=q_sb[:, hc * 128:(hc + 1) * 128], identity=ident_b[:]),
                          r=[Bq, B_const], w=[BpTP])
                fw.act(lambda e, half=half: e.activation(func=AF.Copy, out=qTp[:, half * 8:(half + 1) * 8, :], in_=pTP[:]), r=[BpTP], w=[BqT])
            for n in range(4):
                pq_ = n % 2
                for i in range(4):
                    hc = n * 4 + i
                    fw.pe(lambda e, i=i, hc=hc: e.matmul(pQ[pq_][:, i * 128:(i + 1) * 128], qTp[:, hc, :], sk[:, hc, :], start=True, stop=True,
                                                         skip_group_check=True), r=[BqT, Bw], w=[BpQ[pq_]])
                fw.act(lambda e, n=n: e.activation(func=AF.Copy, out=xap(sc[:, n * 4, :], [[1, 512]]), in_=pQ[pq_][:]), r=[BpQ[pq_]],
                       w=[Bsc[n * 4 + i] for i in range(4)])
            yield
            for rnd in range(2):
                o = rnd * 8
                for hc in range(16):
                    fw.dve(lambda e, hc=hc, o=o: e.max(out=sv[:, hc, o:o + 8], in_=sc[:, hc, :]), r=[Bsc[hc]], w=[Bsv[hc]])
                for hc in range(16):
                    fw.dve(lambda e, hc=hc, o=o: e.max_index(out=si[:, hc, o:o + 8], in_max=sv[:, hc, o:o + 8], in_values=sc[:, hc, :]),
                           r=[Bsc[hc], Bsv[hc]], w=[Bsi[hc]])
                if rnd == 0:
                    for hc in range(16):
                        fw.dve(lambda e, hc=hc: e.match_replace(out=sc[:, hc, :], in_to_replace=sv[:, hc, 0:8], in_values=sc[:, hc, :], imm_value=NEG_SEL),
                               r=[Bsv[hc], Bsc[hc]], w=[Bsc[hc]])
            fw.dve(lambda e: e.tensor_copy(out=sif[:], in_=si[:]), r=Bsi, w=[Bsif])
            yield
            for h in range(8):
                a0 = sv[:, 2 * h, :]; b0 = sv[:, 2 * h + 1, :]
                fw.dve(lambda e, h=h, a0=a0, b0=b0: e.tensor_tensor(out=xap(cand[:, h, :], [[16, 16], [1, 16]]), in0=xap(a0, [[1, 16], [0, 16]]),
                                                                    in1=xap(b0, [[0, 16], [1, 16]]), op=ALU.add),
                       r=[Bsv[2 * h], Bsv[2 * h + 1]], w=[Bcand[h]])
            for rnd in range(2):
                o = rnd * 8
                for h in range(8):
                    fw.dve(lambda e, h=h, o=o: e.max(out=tops[:, h, o:o + 8], in_=cand[:, h, :]), r=[Bcand[h]], w=[Btops[h]])
                for h in range(8):
                    fw.dve(lambda e, h=h, o=o: e.max_index(out=cj[:, h, o:o + 8], in_max=tops[:, h, o:o + 8], in_values=cand[:, h, :]),
                           r=[Bcand[h], Btops[h]], w=[Bcj[h]])
                if rnd == 0:
                    for h in range(8):
                        fw.dve(lambda e, h=h: e.match_replace(out=cand[:, h, :], in_to_replace=tops[:, h, 0:8], in_values=cand[:, h, :], imm_value=NEG_SEL),
                               r=[Btops[h], Bcand[h]], w=[Bcand[h]])
            yield
            fw.dve(lambda e: e.tensor_tensor(out=ex[:], in0=tops[:], in1=xap(tops[:, 0, 0:1], [[16, 8], [0, 16]]), op=ALU.subtract), r=Btops, w=[Bex])
            fw.act(lambda e: e.activation(out=ex[:], in_=ex[:], func=AF.Exp), r=[Bex], w=[Bex])
            fw.dve(lambda e: e.tensor_reduce(out=zz[:, 0:8], in_=ex[:], axis=AX.X, op=ALU.add), r=[Bex], w=[Bex])
            fw.dve(lambda e: e.reciprocal(out=zz[:, 8:16], in_=zz[:, 0:8]), r=[Bex], w=[Bex])
            fw.dve(lambda e: e.tensor_tensor(out=xap(gate[s][:], [[16, 8], [1, 16]]), in0=ex[:], in1=xap(zz[:, 8:16], [[1, 8], [0, 16]]), op=ALU.mult),
                   r=[Bex], w=[Bgate[s]])
            cj_flat = xap(cj[:], [[1, 128]])
            fw.dve(lambda e: e.tensor_single_scalar(out=cji[:, 0, :], in_=cj_flat, scalar=4, op=ALU.logical_shift_right), r=Bcj, w=[Bcjf])
            fw.dve(lambda e: e.tensor_single_scalar(out=cji[:, 1, :], in_=cj_flat, scalar=15, op=ALU.bitwise_and), r=Bcj, w=[Bcjf])
            fw.dve(lambda e: e.tensor_copy(out=cjf[:], in_=cji[:]), r=[Bcjf], w=[Bcjf])
            for ab in range(2):
                yield
                fw.dve(lambda e, ab=ab: e.tensor_tensor(out=ohx[:], in0=xap(io16[:], [[0, 128], [1, 16]]), in1=xap(cjf[:, ab, :], [[1, 128], [0, 16]]),
                                                        op=ALU.is_equal), r=[Bg, Bcjf], w=[Boh])
                fw.dve(lambda e, ab=ab: e.tensor_tensor(out=xap(ohx[:], [[256, 8], [16, 16], [1, 16]]), in0=xap(ohx[:], [[256, 8], [16, 16], [1, 16]]),
                                                        in1=xap(sif[:, ab, :], [[32, 8], [0, 16], [1, 16]]), op=ALU.mult), r=[Boh, Bsif], w=[Boh])
                fw.dve(lambda e, ab=ab: e.tensor_reduce(out=abf[:, ab, :], in_=ohx[:], axis=AX.X, op=ALU.add), r=[Boh], w=[Babf])
            fw.dve(lambda e: e.scalar_tensor_tensor(out=eidx_f[:], in0=abf[:, 0, :], scalar=128.0, in1=abf[:, 1, :], op0=ALU.mult, op1=ALU.add),
                   r=[Babf], w=[Beif])
            fw.dve(lambda e: e.tensor_copy(out=eidx[s][:], in_=eidx_f[:]), r=[Beif], w=[Beidx[s]])

        def experts(j, rgen):
            s = j % 2
            slot_buf = {}

            def grp_dots(g):
                for i in range(GS):
                    sl = g * GS + i
                    b = cn["g"] % NGB; cn["g"] += 1
                    slot_buf[sl] = b
                    fw.dpl(lambda e, sl=sl, b=b: e.indirect_dma_start(out=gb[b][:], out_offset=None, in_=uv_bf,
                                                                      in_offset=bass.IndirectOffsetOnAxis(ap=eidx[s][:, sl:sl + 1], axis=0)),
                           r=[Beidx[s], B_ubf], w=[Bgb[b]])
                    fw.dve(lambda e, sl=sl, b=b: e.scalar_tensor_tensor(out=junk[:], in0=gb[b][:, 0:D], scalar=1.0, in1=h2[s][:], op0=ALU.mult, op1=ALU.mult,
                                                                        accum_out=dots[:, sl:sl + 1]), r=[Bgb[b], Bh2[s]], w=[Bdots[sl]])

            def grp_pre(g):
                c = slice(g * GS, (g + 1) * GS)
                fw.act(lambda e: e.activation(out=tmpa[:, c], in_=dots[:, c], func=AF.Gelu_apprx_tanh), r=Bdots[g * GS:(g + 1) * GS], w=[Btmp[g]])

            def grp_post(g):
                c = slice(g * GS, (g + 1) * GS)
                fw.dve(lambda e: e.tensor_tensor(out=actv[:, c], in0=tmpa[:, c], in1=gate[s][:, c], op=ALU.mult), r=[Btmp[g], Bgate[s]], w=[Bactv[g]])
                for i in range(GS):
                    sl = g * GS + i
                    b = slot_buf[sl]
                    k = cn["d"] % NDG; cn["d"] += 1
                    fw.act(lambda e, sl=sl, k=k: e.activation(out=dg[k][:], in_=ident_b[:], func=AF.Copy, scale=actv[:, sl:sl + 1]),
                           r=[Bactv[g], B_const], w=[Bdg[k]])
                    for half in range(2):
                        fw.pe(lambda e, sl=sl, b=b, k=k, half=half: e.matmul(pA[half][:], dg[k][:], gb[b][:, D + half * 512:D + (half + 1) * 512],
                                                                             start=(sl == 0), stop=(sl == 127)), r=[Bdg[k], Bgb[b]], w=[BpA[half]])

            for g in range(32):
                grp_dots(g)
                grp_pre(g)
                if g >= 1:
                    grp_post(g - 1)
                if rgen is not None and g % 4 == 3:
                    next(rgen, None)
            grp_post(31)
            if rgen is not None:
                for _ in rgen:
                    pass
            for half in range(2):
                fw.dve(lambda e, half=half: e.tensor_tensor(out=x2[:, half * 512:(half + 1) * 512], in0=pA[half][:],
                                                            in1=x1[s][:, half * 512:(half + 1) * 512], op=ALU.add), r=[BpA[half], Bx1[s]], w=[Bx2])
            emit_rms(rt, x2[:], Bx2, 128, gp_sb, Bg, pTP, BpTP, lambda c: h3T[:, c, :], Bh3T)
            fw.act(lambda e: e.activation(func=AF.Copy, out=pb[:], in_=pt_[s][:]), r=[Bpt[s]], w=[Bpb])
            for c2 in range(2):
                fw.pe(lambda e, c2=c2: e.transpose(out=pTP[:, c2, :], in_=pb[:, c2 * 128:(c2 + 1) * 128], identity=ident_b[:]), r=[Bpb, B_const], w=[BpTP])
            fw.act(lambda e: e.activation(func=AF.Copy, out=pT[:], in_=pTP[:, 0:2, :]), r=[BpTP], w=[BpT])
            for half in range(2):
                for c in range(8):
                    fw.pe(lambda e, c=c, half=half: e.matmul(pE[0][:], h3T[:, c, :], wpg[:, c, half * 512:(half + 1) * 512], start=(c == 0), stop=(c == 7)),
                          r=[Bh3T[c], Bw], w=[BpE[0]])
                fw.act(lambda e, half=half: e.activation(out=gsig[:, half * 512:(half + 1) * 512], in_=pE[0][:], func=AF.Sigmoid), r=[BpE[0]], w=[Bgsig])
                for c2 in range(2):
                    fw.pe(lambda e, c2=c2, half=half: e.matmul(pE[1][:], pT[:, c2, :], wpp[:, c2, half * 512:(half + 1) * 512], start=(c2 == 0), stop=(c2 == 1)),
                          r=[BpT, Bw], w=[BpE[1]])
                fw.dve(lambda e, half=half: e.tensor_tensor(out=gsig[:, half * 512:(half + 1) * 512], in0=gsig[:, half * 512:(half + 1) * 512],
                                                            in1=pE[1][:], op=ALU.mult), r=[Bgsig, BpE[1]], w=[Bgsig])
            fw.dve(lambda e: e.tensor_tensor(out=gsig[:], in0=gsig[:], in1=x2[:], op=ALU.add), r=[Bgsig, Bx2], w=[Bgsig])
            fw.dsp(lambda e: e.dma_start(out=out_d[j * 128:(j + 1) * 128, :], in_=gsig[:]), r=[Bgsig], w=[B_out])

        for _ in route(0):
            pass
        for j in range(NJ):
            experts(j, route(j + 1) if j + 1 < NJ else None)
        fw.barrier()
    es_all.close()
    return nc


def _t5_bucket(rel):
    rel = np.asarray(rel, dtype=np.int64)
    half, max_exact = 16, 8
    ret = np.where(rel > 0, half, 0)
    n = np.abs(rel)
    nf = np.maximum(n, 1).astype(np.float32)
    large = max_exact + (np.log(nf / np.float32(max_exact)) / np.float32(math.log(1024 / max_exact)) * np.float32(half - max_exact)).astype(np.int32)
    large = np.minimum(large, half - 1)
    return ret + np.where(n < max_exact, n, large)


_PROG = None


def kernel(x, p, rel_bias, attn_norm_g, w_in, b_gate, q_norm_g, k_norm_g, w_att_out,
           conv_w, conv_b, conv_ln_g, conv_ln_b, w_conv_out, w_out, ffn_norm_g,
           w_peer_q, peer_sub_keys, peer_u, peer_v, ple_norm_g, w_ple_gate, w_ple_proj):
    global _PROG
    f = np.float32
    x = np.asarray(x, f); p = np.asarray(p, f)
    c128 = lambda v, n: np.ascontiguousarray(np.asarray(v, f).reshape(n, 128).T)
    shared = {
        "w_in": np.ascontiguousarray(np.asarray(w_in, f)[0]),
        "rel_bias": np.ascontiguousarray(np.asarray(rel_bias, f)),
        "g_attn": c128(attn_norm_g[0], 8), "g_ffn": c128(ffn_norm_g[0], 8), "g_ple": c128(ple_norm_g[0], 8),
        "g_ffn_bc": np.ascontiguousarray(np.broadcast_to(np.asarray(ffn_norm_g, f)[0][None, :], (128, D))),
        "g_attn_bc": np.ascontiguousarray(np.broadcast_to(np.asarray(attn_norm_g, f)[0][None, :], (128, D))),
        "gq_bc": np.ascontiguousarray(np.broadcast_to(np.tile(np.asarray(q_norm_g, f)[0], 8)[None, :], (128, 512))),
        "gk_bc": np.ascontiguousarray(np.broadcast_to(np.tile(np.asarray(k_norm_g, f)[0], 8)[None, :], (128, 512))),
        "bgate": c128(b_gate[0], 16),
        "w_att_out": np.ascontiguousarray(np.asarray(w_att_out, f)[0]),
        "w_conv_out": np.ascontiguousarray(np.asarray(w_conv_out, f)[0]),
        "w_out": np.ascontiguousarray(np.asarray(w_out, f)[0]),
        "convw": np.ascontiguousarray(np.asarray(conv_w, f)[0][:, 0, :].reshape(31, 4, 128).transpose(2, 1, 0)),
        "convb": c128(conv_b[0], 4), "lng": c128(conv_ln_g[0], 4), "lnb": c128(conv_ln_b[0], 4),
        "w_peer_q": np.ascontiguousarray(np.asarray(w_peer_q, f)[0]),
        "subk": np.ascontiguousarray(np.asarray(peer_sub_keys, f)[0].reshape(16, 128, 128).transpose(2, 0, 1)),
        "peer_u": np.ascontiguousarray(np.asarray(peer_u, f)[0]),
        "peer_v": np.ascontiguousarray(np.asarray(peer_v, f)[0]),
        "w_ple_gate": np.ascontiguousarray(np.asarray(w_ple_gate, f)[0]),
        "w_ple_proj": np.ascontiguousarray(np.asarray(w_ple_proj, f)[0]),
        "ident": np.eye(128, dtype=f),
        "anti": np.ascontiguousarray(np.eye(128, dtype=f)[::-1]),
        "iota16": np.ascontiguousarray(np.broadcast_to(np.arange(16, dtype=f)[None, :], (128, 16))),
        "pw2": np.ascontiguousarray(np.broadcast_to((2.0 ** -(np.arange(32) + 1.0)).astype(f)[None, :], (128, 32))),
    }
    tt = np.arange(128)
    halfmask = np.where((tt[None, :] // 64) <= (tt[:, None] // 64), 0.0, -1.0e9).astype(f)
    in_maps = []
    for core in range(8):
        b, c = core // 2, core % 2
        xb = x[b].reshape(64, 128, D)
        own = xb[c::2]
        halo = np.zeros((NJ, 32, D), f)
        for j in range(NJ):
            blk = 2 * j + c
            if blk > 0:
                halo[j] = xb[blk - 1][96:128]
        idx = np.arange(1536)
        rel = 128 * (1 - c) + 127 - idx
        ohm = (np.arange(32)[:, None] == _t5_bucket(rel)[None, :]).astype(f)
        vm = np.zeros((128, 256), f)
        if c == 0:
            vm[:, 0:128] = halfmask; vm[:, 128:256] = -1.0e9
        else:
            vm[:, 128:256] = halfmask
        m = dict(shared)
        m.update({
            "xs": np.ascontiguousarray(x[b]),
            "xq": np.ascontiguousarray(own.reshape(NT, D)),
            "xh": np.ascontiguousarray(halo.reshape(NJ * 32, D)),
            "pq": np.ascontiguousarray(p[0, b].reshape(64, 128, 256)[c::2].reshape(NT, 256)),
            "oh": ohm, "vm": vm,
        })
        in_maps.append(m)
    if _PROG is None:
        _PROG = build_program()
    res = run_bass_kernel_spmd(_PROG, in_maps, core_ids=list(range(8)))
    out = np.zeros((4, 64, 128, D), f)
    for core in range(8):
        b, c = core // 2, core % 2
        out[b, c::2] = np.asarray(res.results[core]["out"], f).reshape(NJ, 128, D)
    return out.reshape(4, S, D)
```

```python
import math
from contextlib import ExitStack
import numpy as np
import concourse.bass as bass
import concourse.mybir as mybir
from concourse.bass_utils import run_bass_kernel_spmd

F32 = mybir.dt.float32
BF16 = mybir.dt.bfloat16
I32 = mybir.dt.int32
U32 = mybir.dt.uint32
AF = mybir.ActivationFunctionType
ALU = mybir.AluOpType
AX = mybir.AxisListType

D = 1024
S = 8192
NT = 4096
NJ = 32
EPS = 1e-6
NEG_SEL = -3.0e38
MASKV = -30000.0


class Buf:
    __slots__ = ("w", "r")

    def __init__(self):
        self.w = None
        self.r = {}


class Eng:
    def __init__(self, name, obj):
        self.name, self.obj = name, obj
        self.sem = None
        self.key = None
        self.count = 0
        self.epoch = -1
        self.seen = {}


class DmaQ:
    def __init__(self, name, host, nslots):
        self.name, self.host, self.nslots = name, host, nslots
        self.n = 0
        self.sems = [None] * nslots
        self.keys = [None] * nslots
        self.gens = [0] * nslots
        self.epochs = [0] * nslots


class FW:
    EPOCH = 20000
    DGEN = 1500

    def __init__(self, nc, es):
        self.nc, self.es = nc, es
        self.nsem = 0
        self.PE = Eng("pe", nc.tensor)
        self.ACT = Eng("act", nc.scalar)
        self.DVE = Eng("dve", nc.vector)
        self.POOL = Eng("pool", nc.gpsimd)
        self.SP = Eng("sp", nc.sync)
        self.engs = [self.PE, self.ACT, self.DVE, self.POOL, self.SP]
        self.QSP = DmaQ("qsp", self.SP, 6)
        self.QPL = DmaQ("qpl", self.POOL, 6)
        self.QAC = DmaQ("qac", self.ACT, 4)
        self.qs = [self.QSP, self.QPL, self.QAC]
        self.last = {}

    def _newsem(self, name):
        self.nsem += 1
        return self.es.enter_context(self.nc.semaphore(f"{name}_{self.nsem}"))

    def _wait(self, E, rec):
        key, sem, val = rec
        if E.seen.get(key, 0) >= val:
            return
        E.obj.wait_ge(sem, val)
        E.seen[key] = val

    def _deps(self, reads, writes):
        deps = []
        for b in reads:
            if b.w is not None:
                deps.append(b.w)
        for b in writes:
            if b.w is not None:
                deps.append(b.w)
            deps.extend(b.r.values())
        return deps

    def _mark(self, rec, reads, writes):
        for b in reads:
            old = b.r.get(rec[0])
            if old is None or old[2] < rec[2]:
                b.r[rec[0]] = rec
        for b in writes:
            b.w = rec
            b.r = {}
        self.last[rec[0]] = rec

    def op(self, E, fn, reads=(), writes=()):
        if E.sem is None or E.count >= self.EPOCH:
            E.epoch += 1
            E.sem = self._newsem(E.name)
            E.key = f"{E.name}{E.epoch}"
            E.count = 0
        for rec in self._deps(reads, writes):
            if E is self.PE and rec[0].startswith("pe"):
                continue
            self._wait(E, rec)
        inst = fn(E.obj)
        E.count += 1
        inst.then_inc(E.sem, 1)
        rec = (E.key, E.sem, E.count)
        self._mark(rec, reads, writes)
        return rec

    def dma(self, Q, fn, reads=(), writes=()):
        H = Q.host
        for rec in self._deps(reads, writes):
            self._wait(H, rec)
        slot = Q.n % Q.nslots
        if Q.sems[slot] is None or Q.gens[slot] >= self.DGEN:
            if Q.sems[slot] is not None:
                self._wait(H, (Q.keys[slot], Q.sems[slot], 16 * Q.gens[slot]))
            Q.epochs[slot] += 1
            Q.sems[slot] = self._newsem(f"{Q.name}{slot}")
            Q.keys[slot] = f"{Q.name}{slot}e{Q.epochs[slot]}"
            Q.gens[slot] = 0
        if Q.gens[slot] > 0:
            self._wait(H, (Q.keys[slot], Q.sems[slot], 16 * Q.gens[slot]))
        inst = fn(H.obj)
        inst.then_inc(Q.sems[slot], 16)
        Q.gens[slot] += 1
        rec = (Q.keys[slot], Q.sems[slot], 16 * Q.gens[slot])
        Q.n += 1
        self._mark(rec, reads, writes)
        return rec

    def barrier(self):
        recs = list(self.last.values())
        for E in self.engs:
            for rec in recs:
                self._wait(E, rec)

    def pe(self, fn, r=(), w=()):
        return self.op(self.PE, fn, r, w)

    def act(self, fn, r=(), w=()):
        return self.op(self.ACT, fn, r, w)

    def dve(self, fn, r=(), w=()):
        return self.op(self.DVE, fn, r, w)

    def pool(self, fn, r=(), w=()):
        return self.op(self.POOL, fn, r, w)

    def dsp(self, fn, r=(), w=()):
        return self.dma(self.QSP, fn, r, w)

    def dpl(self, fn, r=(), w=()):
        return self.dma(self.QPL, fn, r, w)

    def dac(self, fn, r=(), w=()):
        return self.dma(self.QAC, fn, r, w)


def xap(ap, pat):
    return bass.AP(ap.tensor, ap.offset, [list(ap.ap[0])] + [list(p) for p in pat])


def build_program():
    nc = bass.Bass("TRN2", target_bir_lowering=False)

    def din(name, shape, dt=F32):
        return nc.dram_tensor(name, list(shape), dt, kind="ExternalInput").ap()

    xs = din("xs", [S, D]); xq = din("xq", [NT, D]); xh = din("xh", [NJ * 32, D]); pq = din("pq", [NT, 256])
    w_in = din("w_in", [D, 5192])
    relb = din("rel_bias", [32, 8]); oh = din("oh", [32, 1536])
    g_attn = din("g_attn", [128, 8]); g_ffn = din("g_ffn", [128, 8]); g_ple = din("g_ple", [128, 8])
    g_ffn_bc = din("g_ffn_bc", [128, D]); g_attn_bc = din("g_attn_bc", [128, D])
    gq_bc = din("gq_bc", [128, 512]); gk_bc = din("gk_bc", [128, 512])
    bgate = din("bgate", [128, 16])
    w_ao = din("w_att_out", [512, D]); w_co = din("w_conv_out", [512, D]); w_out = din("w_out", [D, D])
    convw = din("convw", [128, 4, 31]); convb = din("convb", [128, 4]); lng = din("lng", [128, 4]); lnb = din("lnb", [128, 4])
    w_pq = din("w_peer_q", [D, 2048]); subk = din("subk", [128, 16, 128])
    peer_u = din("peer_u", [16384, D]); peer_v = din("peer_v", [16384, D])
    w_pg = din("w_ple_gate", [D, D]); w_pp = din("w_ple_proj", [256, D])
    ident_d = din("ident", [128, 128]); anti_d = din("anti", [128, 128]); vm_d = din("vm", [128, 256])
    iota16_d = din("iota16", [128, 16])
    pw2_d = din("pw2", [128, 32])
    out_d = nc.dram_tensor("out", [NT, D], F32, kind="ExternalOutput").ap()
    kTs_t = nc.dram_tensor("kTs", [512, S], BF16, kind="Internal"); kTs = kTs_t.ap()
    vs_t = nc.dram_tensor("vs", [S, 520], BF16, kind="Internal"); vs = vs_t.ap()
    gtab_t = nc.dram_tensor("gtab", [8, 1536], BF16, kind="Internal"); gtab = gtab_t.ap()
    x1s_t = nc.dram_tensor("x1s", [NT, D], F32, kind="Internal"); x1s = x1s_t.ap()
    attTs_t = nc.dram_tensor("attTs", [512, NT], BF16, kind="Internal")
    uv_bf = nc.dram_tensor("uv_bf", [16384, 2 * D], BF16, kind="Internal").ap()
    B_ubf = Buf()

    es_all = ExitStack()
    fw = FW(nc, es_all)

    def wview(w, c0, c1):
        return w[:, c0:c1].rearrange("(c p) n -> p c n", p=128)

    def SB(es, name, shape, dt):
        return es.enter_context(nc.sbuf_tensor(name, list(shape), dt))

    def PS(es, name, shape, dt=F32):
        return es.enter_context(nc.psum_tensor(name, list(shape), dt))

    ident_f = SB(es_all, "ident_f", [128, 128], F32)
    ident_b = SB(es_all, "ident_b", [128, 128], BF16)
    anti_b = SB(es_all, "anti_b", [128, 128], BF16)
    ones_f = SB(es_all, "ones_f", [128, 128], F32)
    B_const = Buf()
    B_attT = [Buf() for _ in range(NJ)]
    fw.dsp(lambda e: e.dma_start(out=ident_f[:], in_=ident_d), w=[B_const])
    fw.dpl(lambda e: e.dma_start(out=ident_b[:], in_=ident_d), w=[B_const])
    fw.dpl(lambda e: e.dma_start(out=anti_b[:], in_=anti_d), w=[B_const])
    fw.pool(lambda e: e.memset(ones_f[:], 1.0), w=[B_const])

    def rms_hT(es_tmp, tag):
        t = {}
        t["ssq"] = SB(es_tmp, f"rs_{tag}", [128, 4], F32)
        t["xn"] = SB(es_tmp, f"rx_{tag}", [128, D], BF16)
        t["B"] = Buf()
        t["Bch"] = Buf()
        return t

    def emit_rms(t, xt, Bx, n, g_sb, Bg, psT, BpsT, hT_ap_fn, BhT, gbc=None, h_f32=None, Bh32=None, hT_all=None, phase="ab"):
        fused = gbc is not None and hT_all is not None
        if "a" in phase:
            fw.act(lambda e: e.activation(out=t["xn"][0:n, :], in_=xt, func=AF.Square, accum_out=t["ssq"][0:n, 0:1]), r=[Bx], w=[t["B"]])
            fw.act(lambda e: e.activation(out=t["ssq"][0:n, 1:2], in_=t["ssq"][0:n, 0:1], func=AF.Sqrt, scale=1.0 / D, bias=EPS),
                   r=[t["B"]], w=[t["B"]])
            fw.dve(lambda e: e.reciprocal(out=t["ssq"][0:n, 2:3], in_=t["ssq"][0:n, 1:2]), r=[t["B"]], w=[t["B"]])
            if fused:
                fw.dve(lambda e: e.scalar_tensor_tensor(out=t["xn"][0:n, :], in0=xt, scalar=t["ssq"][0:n, 2:3], in1=gbc[0:n, :],
                                                        op0=ALU.mult, op1=ALU.mult), r=[Bx, t["B"], Bg], w=[t["B"]])
            else:
                fw.dve(lambda e: e.tensor_scalar(out=t["xn"][0:n, :], in0=xt, scalar1=t["ssq"][0:n, 2:3], scalar2=None, op0=ALU.mult),
                       r=[Bx, t["B"]], w=[t["B"]])
            if h_f32 is not None:
                fw.dve(lambda e: e.scalar_tensor_tensor(out=h_f32, in0=xt, scalar=t["ssq"][0:n, 2:3], in1=gbc,
                                                        op0=ALU.mult, op1=ALU.mult), r=[Bx, t["B"], Bg], w=[Bh32])
        if "b" not in phase:
            return
        for c in range(8):
            fw.pe(lambda e, c=c: e.transpose(out=psT[:, c, 0:n], in_=t["xn"][0:n, c * 128:(c + 1) * 128], identity=ident_b[0:n, 0:n]),
                  r=[t["B"], B_const], w=[BpsT])
        if fused:
            fw.act(lambda e: e.activation(out=hT_all, in_=psT[:, :, 0:n], func=AF.Copy), r=[BpsT], w=list(BhT))
        else:
            for c in range(8):
                if c % 2 == 0:
                    fw.act(lambda e, c=c: e.activation(out=hT_ap_fn(c), in_=psT[:, c, 0:n], func=AF.Copy, scale=g_sb[:, c:c + 1]),
                           r=[BpsT, Bg], w=[BhT[c], t["Bch"]])
                else:
                    fw.dve(lambda e, c=c: e.tensor_scalar(out=hT_ap_fn(c), in0=psT[:, c, 0:n], scalar1=g_sb[:, c:c + 1], scalar2=None, op0=ALU.mult),
                           r=[BpsT, Bg], w=[BhT[c], t["Bch"]])

    def emit_qknorm(tq, ps_ap, Bps, gbc, Bg, scale, out_bf, Bout):
        fw.act(lambda e: e.activation(func=AF.Copy, out=tq["f"][:], in_=ps_ap), r=[Bps], w=[tq["B"]])
        fw.dve(lambda e: e.tensor_tensor(out=tq["sq"][:], in0=tq["f"][:], in1=tq["f"][:], op=ALU.mult), r=[tq["B"]], w=[tq["B2"]])
        fw.dve(lambda e: e.tensor_reduce(out=tq["s8"][:, 0:8], in_=xap(tq["sq"][:], [[64, 8], [1, 64]]), axis=AX.X, op=ALU.add),
               r=[tq["B2"]], w=[tq["B3"]])
        fw.act(lambda e: e.activation(out=tq["s8"][:, 8:16], in_=tq["s8"][:, 0:8], func=AF.Sqrt, scale=1.0 / 64, bias=EPS),
               r=[tq["B3"]], w=[tq["B3"]])
        fw.dve(lambda e: e.reciprocal(out=tq["s8"][:, 16:24], in_=tq["s8"][:, 8:16]), r=[tq["B3"]], w=[tq["B3"]])
        fw.dve(lambda e: e.tensor_tensor(out=xap(tq["sq"][:], [[64, 8], [1, 64]]), in0=xap(tq["f"][:], [[64, 8], [1, 64]]),
                                         in1=xap(tq["s8"][:, 16:24], [[1, 8], [0, 64]]), op=ALU.mult),
               r=[tq["B"], tq["B3"]], w=[tq["B2"]])
        fw.dve(lambda e: e.scalar_tensor_tensor(out=out_bf, in0=tq["sq"][:], scalar=float(scale), in1=gbc, op0=ALU.mult, op1=ALU.mult),
               r=[tq["B2"], Bg], w=[Bout])

    def mk_qk(es, tag):
        return {"f": SB(es, f"qf_{tag}", [128, 512], F32), "sq": SB(es, f"qs_{tag}", [128, 512], F32),
                "s8": SB(es, f"q8_{tag}", [128, 24], F32), "B": Buf(), "B2": Buf(), "B3": Buf()}

    with ExitStack() as es:
        g_sb = SB(es, "g_attn_sb", [128, 8], F32); Bg = Buf()
        gq_sb = SB(es, "gq_sb", [128, 512], F32); gk_sb = SB(es, "gk_sb", [128, 512], F32)
        vm_sb = SB(es, "vm_sb", [128, 256], F32)
        gabc = SB(es, "gabc", [128, D], F32)
        fw.dsp(lambda e: e.dma_start(out=gabc[:], in_=g_attn_bc), w=[Bg])
        pw_sb = SB(es, "pw_sb", [128, 32], F32)
        cbias = SB(es, "cbias", [128, 8], F32)
        fw.dsp(lambda e: e.dma_start(out=cbias[:], in_=bass.AP(relb.tensor, 15 * 8, [[0, 128], [1, 8]])), w=[Bg])
        fw.dsp(lambda e: e.dma_start(out=pw_sb[:], in_=pw2_d), w=[Bg])
        fw.dsp(lambda e: e.dma_start(out=g_sb[:], in_=g_attn), w=[Bg])
        fw.dsp(lambda e: e.dma_start(out=gq_sb[:], in_=gq_bc), w=[Bg])
        fw.dsp(lambda e: e.dma_start(out=gk_sb[:], in_=gk_bc), w=[Bg])
        fw.dsp(lambda e: e.dma_start(out=vm_sb[:], in_=vm_d), w=[Bg])
        kiT = SB(es, "kiT", [128, S], BF16); B_ki = [Buf() for _ in range(64)]
        TB = SB(es, "TB", [128, 8, 11, 128], BF16); B_TB = Buf()
        with ExitStack() as es2:
            rb_sb = SB(es2, "rb_sb", [32, 8], F32); oh_sb = SB(es2, "oh_sb", [32, 1536], F32)
            g_bf = SB(es2, "g_bf", [8, 1536], BF16)
            psg = PS(es2, "psg", [8, 512]); Bt = Buf(); Bp = Buf(); Bgt = Buf()
            fw.dsp(lambda e: e.dma_start(out=rb_sb[:], in_=relb), w=[Bt])
            fw.dsp(lambda e: e.dma_start(out=oh_sb[:], in_=oh), w=[Bt])
            for i in range(3):
                fw.pe(lambda e, i=i: e.matmul(psg[:], rb_sb[:], oh_sb[:, i * 512:(i + 1) * 512], start=True, stop=True), r=[Bt], w=[Bp])
                fw.act(lambda e, i=i: e.activation(func=AF.Copy, out=g_bf[:, i * 512:(i + 1) * 512], in_=psg[:]), r=[Bp], w=[Bgt])
            fw.dsp(lambda e: e.dma_start(out=gtab, in_=g_bf[:]), r=[Bgt], w=[B_TB])
            for h in range(8):
                for m in range(11):
                    ip = 10 - m
                    src = bass.AP(gtab_t, h * 1536 + 128 * ip, [[1, 128], [1, 128]])
                    fw.dsp(lambda e, h=h, m=m, src=src: e.dma_start(out=TB[:, h, m, :], in_=src), r=[B_TB], w=[Buf()])
            fw.barrier()

        with ExitStack() as es2:
            wk = SB(es2, "wk", [128, 8, 512], BF16); wv = SB(es2, "wv", [128, 8, 512], BF16)
            wki = SB(es2, "wki", [128, 8, 128], BF16); Bw = Buf()
            fw.dpl(lambda e: e.dma_start(out=wk[:], in_=wview(w_in, 512, 1024)), w=[Bw])
            fw.dpl(lambda e: e.dma_start(out=wv[:], in_=wview(w_in, 1024, 1536)), w=[Bw])
            fw.dpl(lambda e: e.dma_start(out=wki[:, :, 0:64], in_=wview(w_in, 2048, 2112)), w=[Bw])
            fw.dpl(lambda e: e.dma_start(out=wki[:, :, 64:128], in_=wview(w_in, 2048, 2112)), w=[Bw])
            xt = [SB(es2, f"xtA{i}", [128, D], F32) for i in range(3)]; Bxt = [Buf() for _ in range(3)]
            rtA = [rms_hT(es2, "A0"), rms_hT(es2, "A1")]
            hT = [SB(es2, f"hTA{i}", [128, 8, 128], BF16) for i in range(2)]; BhT = [[Buf() for _ in range(8)], [Buf() for _ in range(8)]]
            psT = PS(es2, "psTA", [128, 8, 128], BF16); BpsT = Buf()
            pk = PS(es2, "pkA", [128, 512]); Bpk = Buf()
            pv = PS(es2, "pvA", [128, 512]); Bpv = Buf()
            pki = PS(es2, "pkiA", [128, 128]); Bpki = Buf()
            pkt = PS(es2, "pktA", [128, 4, 128], BF16); Bpkt = Buf()
            tq = mk_qk(es2, "A")
            kn = [SB(es2, f"knA{i}", [128, 512], BF16) for i in range(2)]; Bkn = [Buf(), Buf()]
            kt_sb = [SB(es2, f"ktA{i}", [128, 4, 128], BF16) for i in range(2)]; Bkt = [Buf(), Buf()]
            v_sb = [SB(es2, f"vA{i}", [128, 8, 65], BF16) for i in range(2)]; Bv = [Buf(), Buf()]
            for i in range(2):
                fw.dve(lambda e, i=i: e.memset(v_sb[i][:], 1.0), w=[Bv[i]])
            B_kTs = Buf(); B_vs = Buf()

            def A0(kb):
                fw.dsp(lambda e: e.dma_start(out=xt[kb % 3][:], in_=xs[kb * 128:(kb + 1) * 128, :]), w=[Bxt[kb % 3]])

            def A1a(kb):
                s = kb % 2
                emit_rms(rtA[s], xt[kb % 3][:], Bxt[kb % 3], 128, g_sb, Bg, psT, BpsT, lambda c: hT[s][:, c, :], BhT[s], gbc=gabc[:], hT_all=hT[s][:],
                         phase="a")

            def A1(kb):
                s = kb % 2
                emit_rms(rtA[s], xt[kb % 3][:], Bxt[kb % 3], 128, g_sb, Bg, psT, BpsT, lambda c: hT[s][:, c, :], BhT[s], gbc=gabc[:], hT_all=hT[s][:],
                         phase="b")

            def A2a(kb):
                s = kb % 2
                for c in range(8):
                    fw.pe(lambda e, c=c: e.matmul(pk[:], hT[s][:, c, :], wk[:, c, :], start=(c == 0), stop=(c == 7)), r=[BhT[s][c], Bw], w=[Bpk])
                for c in range(8):
                    fw.pe(lambda e, c=c: e.matmul(pv[:], hT[s][:, c, :], wv[:, c, :], start=(c == 0), stop=(c == 7)), r=[BhT[s][c], Bw], w=[Bpv])
                for c in range(8):
                    fw.pe(lambda e, c=c: e.matmul(pki[:], wki[:, c, :], hT[s][:, c, :], start=(c == 0), stop=(c == 7)), r=[BhT[s][c], Bw], w=[Bpki])
                fw.act(lambda e: e.activation(func=AF.Copy, out=kiT[:, kb * 128:(kb + 1) * 128], in_=pki[:]), r=[Bpki], w=[B_ki[kb]])
                fw.act(lambda e: e.activation(func=AF.Copy, out=v_sb[s][:, :, 0:64], in_=xap(pv[:], [[64, 8], [1, 64]])), r=[Bpv], w=[Bv[s]])
                fw.dac(lambda e: e.dma_start(out=vs[kb * 128:(kb + 1) * 128, :], in_=xap(v_sb[s][:], [[1, 520]])), r=[Bv[s]], w=[Buf()])
                emit_qknorm(tq, pk[:], Bpk, gk_sb[:], Bg, 1.0, kn[s][:], Bkn[s])

            def A2b(kb):
                s = kb % 2
                for pr in range(4):
                    fw.pe(lambda e, pr=pr: e.transpose(out=pkt[:, pr, :], in_=kn[s][:, pr * 128:(pr + 1) * 128], identity=ident_b[:]),
                          r=[Bkn[s], B_const], w=[Bpkt])
                fw.act(lambda e: e.activation(func=AF.Copy, out=kt_sb[s][:], in_=pkt[:]), r=[Bpkt], w=[Bkt[s]])
                dst = bass.AP(kTs_t, kb * 128, [[S, 128], [128 * S, 4], [1, 128]])
                fw.dac(lambda e: e.dma_start(out=dst, in_=kt_sb[s][:]), r=[Bkt[s]], w=[Buf()])

            A0(0); A0(1)
            A1a(0); A1(0); A1a(1)
            for kb in range(64):
                if kb + 2 < 64:
                    A0(kb + 2)
                if kb + 1 < 64:
                    A1(kb + 1)
                if kb + 2 < 64:
                    A1a(kb + 2)
                A2a(kb)
                if kb >= 1:
                    A2b(kb - 1)
            A2b(63)
            fw.barrier()

        with ExitStack() as es2:
            wq = SB(es2, "wq", [128, 8, 512], BF16); wqi = SB(es2, "wqi", [128, 8, 512], BF16)
            wwi = SB(es2, "wwi", [128, 8, 8], BF16); Bw = Buf()
            fw.dpl(lambda e: e.dma_start(out=wq[:], in_=wview(w_in, 0, 512)), w=[Bw])
            fw.dpl(lambda e: e.dma_start(out=wqi[:], in_=wview(w_in, 1536, 2048)), w=[Bw])
            fw.dpl(lambda e: e.dma_start(out=wwi[:], in_=wview(w_in, 2112, 2120)), w=[Bw])
            xt = [SB(es2, f"xtB{i}", [128, D], F32) for i in range(2)]; Bxt = [Buf(), Buf()]
            rtB = [rms_hT(es2, "B0"), rms_hT(es2, "B1")]
            hT = SB(es2, "hTB", [128, 8, 128], BF16); BhT = [Buf() for _ in range(8)]
            tq = mk_qk(es2, "B")
            qn = SB(es2, "qnB", [128, 512], BF16); Bqn = Buf()
            qib = SB(es2, "qib", [128, 512], BF16); Bqib = Buf()
            qT = [SB(es2, f"qT{i}", [128, 4, 128], BF16) for i in range(2)]; BqT = [Buf(), Buf()]
            qiT = SB(es2, "qiT", [128, 4, 128], BF16); BqiT = Buf()
            wi_sb = SB(es2, "wi_sb", [128, 8], F32); Bwi = Buf()
            NRL = 4
            relu_sb = [SB(es2, f"relu{i}", [128, 512], BF16) for i in range(NRL)]; Brelu = [Buf() for _ in range(NRL)]
            scores = SB(es2, "scores", [128, S], F32); Bsc = Buf()
            tk = SB(es2, "tk", [128, 8], F32); Btk = Buf(); Blo = Buf(); Bw0 = Buf(); Bmid = Buf(); Bcnt = Buf(); Bc = Buf()
            am = SB(es2, "am", [128, 16], F32); Bam = Buf()
            wt = SB(es2, "wt", [128, 32], F32); Bwt = Buf()
            cu = SB(es2, "cu", [128, 2], U32)
            madd = [SB(es2, f"madd{i}", [128, S], BF16) for i in range(2)]; Bmadd = [Buf(), Buf()]
            NSL = 2
            kTc = [SB(es2, f"kTc{i}", [128, 4, 512], BF16) for i in range(NSL)]; BkTc = [Buf() for _ in range(NSL)]
            vc = [SB(es2, f"vc{i}", [128, 4, 520], BF16) for i in range(NSL)]; Bvc = [Buf() for _ in range(NSL)]
            PT = [SB(es2, f"PT{i}", [128, 4, 128], BF16) for i in range(3)]; BPT = [Buf() for _ in range(3)]
            rden = SB(es2, "rden", [128, 8], F32); Brd = Buf()
            att = SB(es2, "att", [128, 512], BF16); Batt = Buf()
            attTb = SB(es2, "attTb", [128, 4, 128], BF16); BattTb = Buf()
            pTP = PS(es2, "pTP", [128, 8, 128], BF16); BpTP = Buf()
            pD = [PS(es2, f"pD{i}", [128, 512]) for i in range(2)]; BpD = [Buf(), Buf()]
            pS = PS(es2, "pS", [128, 512]); BpS = Buf()
            pP = pS; BpP = BpS
            pST = [PS(es2, f"pST{i}", [128, 4, 128]) for i in range(2)]; BpST = [Buf(), Buf()]
            pO = [PS(es2, f"pO{i}", [128, 4, 65]) for i in range(2)]; BpO = [Buf(), Buf()]

            def prologue_a(j):
                s = j % 2
                fw.dsp(lambda e: e.dma_start(out=xt[s][:], in_=xq[j * 128:(j + 1) * 128, :]), w=[Bxt[s]])
                emit_rms(rtB[s], xt[s][:], Bxt[s], 128, g_sb, Bg, pTP, BpTP, lambda c: hT[:, c, :], BhT, gbc=gabc[:], hT_all=hT[:], phase="a")

            def prologue(j):
                s = j % 2
                emit_rms(rtB[s], xt[s][:], Bxt[s], 128, g_sb, Bg, pTP, BpTP, lambda c: hT[:, c, :], BhT, gbc=gabc[:], hT_all=hT[:], phase="b")
                for c in range(8):
                    fw.pe(lambda e, c=c: e.matmul(pP[:], hT[:, c, :], wq[:, c, :], start=(c == 0), stop=(c == 7)), r=[BhT[c], Bw], w=[BpP])
                emit_qknorm(tq, pP[:], BpP, gq_sb[:], Bg, 0.125, qn[:], Bqn)
                for c in range(8):
                    fw.pe(lambda e, c=c: e.matmul(pP[:], hT[:, c, :], wqi[:, c, :], start=(c == 0), stop=(c == 7)), r=[BhT[c], Bw], w=[BpP])
                fw.act(lambda e: e.activation(func=AF.Copy, out=qib[:], in_=pP[:]), r=[BpP], w=[Bqib])
                for pr in range(4):
                    fw.pe(lambda e, pr=pr: e.transpose(out=pTP[:, pr, :], in_=qib[:, pr * 128:(pr + 1) * 128], identity=ident_b[:]),
                          r=[Bqib, B_const], w=[BpTP])
                fw.act(lambda e: e.activation(func=AF.Copy, out=qiT[:], in_=pTP[:, 0:4, :]), r=[BpTP], w=[BqiT])
                for c in range(8):
                    fw.pe(lambda e, c=c: e.matmul(pP[:, 0:8], hT[:, c, :], wwi[:, c, :], start=(c == 0), stop=(c == 7)), r=[BhT[c], Bw], w=[BpP])
                fw.act(lambda e: e.activation(func=AF.Copy, out=wi_sb[:], in_=pP[:, 0:8]), r=[BpP], w=[Bwi])

            def prologue_q(j):
                s = j % 2
                for pr in range(4):
                    fw.pe(lambda e, pr=pr: e.transpose(out=pTP[:, pr, :], in_=qn[:, pr * 128:(pr + 1) * 128], identity=ident_b[:]),
                          r=[Bqn, B_const], w=[BpTP])
                fw.act(lambda e: e.activation(func=AF.Copy, out=qT[s][:], in_=pTP[:, 0:4, :]), r=[BpTP], w=[BqT[s]])

            cnt = {"relu": 0, "d": 0}

            def indexer(j):
                NK = (2 * j + 2) * 128
                nkt = (NK + 511) // 512
                for kt in range(nkt):
                    k0 = kt * 512
                    w = min(512, NK - k0)
                    kbs = [B_ki[b] for b in range(k0 // 128, (k0 + w) // 128)]
                    for h in range(8):
                        d = cnt["d"] % 2; cnt["d"] += 1
                        hp = h % 2
                        fw.pe(lambda e: e.matmul(pD[d][:, 0:w], qiT[hp * 64:(hp + 1) * 64, h // 2, :], kiT[hp * 64:(hp + 1) * 64, k0:k0 + w],
                                                 start=True, stop=True), r=[BqiT] + kbs, w=[BpD[d]])
                        rs = cnt["relu"] % NRL; cnt["relu"] += 1
                        fw.act(lambda e: e.activation(out=relu_sb[rs][:, 0:w], in_=pD[d][:, 0:w], func=AF.Relu), r=[BpD[d]], w=[Brelu[rs]])
                        if h == 0:
                            fw.dve(lambda e: e.tensor_scalar(out=scores[:, k0:k0 + w], in0=relu_sb[rs][:, 0:w], scalar1=wi_sb[:, 0:1], scalar2=None,
                                                             op0=ALU.mult), r=[Brelu[rs], Bwi], w=[Bsc])
                        else:
                            fw.dve(lambda e: e.scalar_tensor_tensor(out=scores[:, k0:k0 + w], in0=relu_sb[rs][:, 0:w], scalar=wi_sb[:, h:h + 1],
                                                                    in1=scores[:, k0:k0 + w], op0=ALU.mult, op1=ALU.add),
                                   r=[Brelu[rs], Bwi, Bsc], w=[Bsc])
                fw.dve(lambda e: e.tensor_tensor(out=scores[:, NK - 256:NK], in0=scores[:, NK - 256:NK], in1=vm_sb[:], op=ALU.add),
                       r=[Bsc, Bg], w=[Bsc])

            NIT = 24

            def topk(j):
                NK = (2 * j + 2) * 128
                nkt = (NK + 511) // 512
                ms = j % 2
                if j == 0:
                    fw.dve(lambda e: e.tensor_scalar(out=madd[ms][:, 0:NK], in0=scores[:, 0:NK], scalar1=-1.0e8, scalar2=MASKV,
                                                     op0=ALU.is_le, op1=ALU.mult), r=[Bsc], w=[Bmadd[ms]])
                    return
                fw.dve(lambda e: e.tensor_reduce(out=tk[:, 0:1], in_=scores[:, 0:NK - 256], axis=AX.X, op=ALU.max, apply_absolute_value=True),
                       r=[Bsc], w=[Btk])
                fw.dve(lambda e: e.tensor_scalar(out=tk[:, 2:3], in0=tk[:, 0:1], scalar1=2.002, scalar2=2.0e-6, op0=ALU.mult, op1=ALU.add), r=[Btk], w=[Bw0])
                fw.dve(lambda e: e.tensor_scalar(out=wt[:], in0=pw_sb[:], scalar1=tk[:, 2:3], scalar2=None, op0=ALU.mult), r=[Bw0, Bg], w=[Bwt])
                fw.dve(lambda e: e.tensor_scalar(out=tk[:, 1:2], in0=tk[:, 0:1], scalar1=-1.001, scalar2=-1.0e-6, op0=ALU.mult, op1=ALU.add), r=[Btk], w=[Blo])
                Bj = Buf()
                for it in range(NIT):
                    fw.dve(lambda e, it=it: e.tensor_tensor(out=tk[:, 3:4], in0=tk[:, 1:2], in1=wt[:, it:it + 1], op=ALU.add), r=[Blo, Bwt], w=[Bmid])
                    fw.dve(lambda e: e.tensor_scalar(out=madd[ms][:, 0:NK], in0=scores[:, 0:NK], scalar1=tk[:, 3:4], scalar2=0.0,
                                                     op0=ALU.is_ge, op1=ALU.add, accum_out=tk[:, 4:5]),
                           r=[Bsc, Bmid], w=([Bmadd[ms], Bj, Bcnt] if it == 0 else [Bj, Bcnt]))
                    fw.dve(lambda e: e.memset(tk[:, 6:7], 0.0), w=[Bcnt])
                    fw.dve(lambda e: e.tensor_scalar(out=cu[:, 0:1], in0=tk[:, 4:5], scalar1=255.5, scalar2=None, op0=ALU.is_ge), r=[Bcnt], w=[Bc])
                    fw.dve(lambda e: e.copy_predicated(out=tk[:, 1:2], mask=cu[:, 0:1], data=tk[:, 3:4]), r=[Bc, Bmid], w=[Blo])
                fw.dve(lambda e: e.tensor_scalar(out=madd[ms][:, 0:NK], in0=scores[:, 0:NK], scalar1=tk[:, 1:2], scalar2=MASKV,
                                                 op0=ALU.is_lt, op1=ALU.mult), r=[Bsc, Blo], w=[Bmadd[ms], Bj])

            acnt = {"g": 0, "st": 0, "pt": 0}

            def attention(j):
                NB = 2 * j + 2
                NG = (NB + 3) // 4
                s = j % 2
                ms = j % 2
                for i in range(2):
                    fw.dve(lambda e, i=i: e.memset(pO[i][:], 0.0), w=[BpO[i]])
                steps = [(g, h) for g in range(NG) for h in range(8)]
                ginfo = {}
                sinfo = {}

                def load_group(g):
                    nb = min(4, NB - 4 * g)
                    sl = acnt["g"] % NSL; acnt["g"] += 1
                    k0 = g * 512
                    srck = bass.AP(kTs_t, k0, [[S, 128], [128 * S, 4], [1, nb * 128]])
                    fw.dsp(lambda e: e.dma_start(out=kTc[sl][:, :, 0:nb * 128], in_=srck), r=[B_kTs], w=[BkTc[sl]])
                    srcv = bass.AP(vs_t, k0 * 520, [[520, 128], [128 * 520, nb], [1, 520]])
                    fw.dsp(lambda e: e.dma_start(out=vc[sl][:, 0:nb, :], in_=srcv), r=[B_vs], w=[Bvc[sl]])
                    ip0 = (2 * j + 1) - 4 * g
                    ginfo[g] = (nb, sl, ip0, max(0, 10 - ip0))

                def S_step(k):
                    g, h = steps[k]
                    if h == 0:
                        load_group(g)
                    nb, sl, ip0, m0 = ginfo[g]
                    st = acnt["st"] % 2; acnt["st"] += 1
                    pt = acnt["pt"] % 3; acnt["pt"] += 1
                    sinfo[k] = pt
                    hp = h % 2
                    all_far = (ip0 - (nb - 1)) >= 7
                    if not all_far:
                        fw.pe(lambda e: e.matmul(pST[st][:, 0:nb, :], anti_b[:], TB[:, h, m0:m0 + nb, :], start=True, stop=False,
                                                 skip_group_check=True), r=[B_const, B_TB], w=[BpST[st]])
                    for b in range(nb):
                        fw.pe(lambda e, b=b: e.matmul(pST[st][:, b, :], kTc[sl][hp * 64:(hp + 1) * 64, h // 2, b * 128:(b + 1) * 128],
                                                      qT[s][hp * 64:(hp + 1) * 64, h // 2, :], start=(all_far and b == 0), stop=False,
                                                      skip_group_check=True),
                              r=[BkTc[sl], BqT[s]], w=[BpST[st]])
                        fw.pe(lambda e, b=b: e.matmul(pST[st][:, b, :], madd[ms][:, (4 * g + b) * 128:(4 * g + b + 1) * 128], ident_b[:],
                                                      start=False, stop=(b == nb - 1), skip_group_check=True),
                              r=[Bmadd[ms], B_const], w=[BpST[st]])
                    if all_far:
                        fw.act(lambda e: e.activation(out=PT[pt][:, 0:nb, :], in_=pST[st][:, 0:nb, :], func=AF.Exp, bias=cbias[:, h:h + 1]),
                               r=[BpST[st], Bg], w=[BPT[pt]])
                    else:
                        fw.act(lambda e: e.activation(out=PT[pt][:, 0:nb, :], in_=pST[st][:, 0:nb, :], func=AF.Exp), r=[BpST[st]], w=[BPT[pt]])

                def PV_step(k):
                    g, h = steps[k]
                    nb, sl, ip0, m0 = ginfo[g]
                    pt = sinfo[k]
                    for b in range(nb):
                        fw.pe(lambda e, b=b: e.matmul(pO[h // 4][:, h % 4, :], PT[pt][:, b, :], vc[sl][:, b, h * 65:(h + 1) * 65], start=False, stop=False,
                                                      skip_group_check=True), r=[BPT[pt], Bvc[sl]], w=[BpO[h // 4]])

                S_step(0)
                for k in range(len(steps)):
                    if k + 1 < len(steps):
                        S_step(k + 1)
                    PV_step(k)

            def attention_epilogue(j):
                for i in range(2):
                    fw.dve(lambda e, i=i: e.reciprocal(out=rden[:, 4 * i:4 * i + 4], in_=xap(pO[i][:, 0, 64:65], [[65, 4]])), r=[BpO[i]], w=[Brd])
                for i in range(2):
                    fw.dve(lambda e, i=i: e.tensor_tensor(out=xap(att[:, 256 * i:256 * i + 256], [[64, 4], [1, 64]]), in0=pO[i][:, :, 0:64],
                                                          in1=xap(rden[:, 4 * i:4 * i + 4], [[1, 4], [0, 64]]), op=ALU.mult),
                           r=[BpO[i], Brd], w=[Batt])
                for pr in range(4):
                    fw.pe(lambda e, pr=pr: e.transpose(out=pTP[:, 4 + pr, :], in_=att[:, pr * 128:(pr + 1) * 128], identity=ident_b[:]),
                          r=[Batt, B_const], w=[BpTP])
                fw.act(lambda e: e.activation(func=AF.Copy, out=attTb[:], in_=pTP[:, 4:8, :]), r=[BpTP], w=[BattTb])
                dsta = bass.AP(attTs_t, j * 128, [[NT, 128], [128 * NT, 4], [1, 128]])
                fw.dac(lambda e: e.dma_start(out=dsta, in_=attTb[:]), r=[BattTb], w=[B_attT[j]])

            prologue_a(0); prologue(0); indexer(0); prologue_q(0); topk(0)
            prologue_a(1)
            for j in range(NJ):
                if j + 1 < NJ:
                    prologue(j + 1); indexer(j + 1); prologue_q(j + 1)
                if j >= 1:
                    attention_epilogue(j - 1)
                if j + 2 < NJ:
                    prologue_a(j + 2)
                attention(j)
                if j + 1 < NJ:
                    topk(j + 1)
            attention_epilogue(NJ - 1)
            fw.barrier()

    with ExitStack() as es:
        g_sb = SB(es, "g_attn_sb2", [128, 8], F32); Bg = Buf()
        bg_sb = SB(es, "bg_sb", [128, 16], F32)
        gabc2 = SB(es, "gabc2", [128, D], F32)
        fw.dsp(lambda e: e.dma_start(out=gabc2[:], in_=g_attn_bc), w=[Bg])
        cw = SB(es, "cw", [128, 4, 31], F32); cb = SB(es, "cb", [128, 4], F32)
        lg = SB(es, "lg", [128, 4], F32); lb = SB(es, "lb", [128, 4], F32)
        for dst_, src_ in ((g_sb, g_attn), (bg_sb, bgate), (cw, convw), (cb, convb), (lg, lng), (lb, lnb)):
            fw.dsp(lambda e, dst_=dst_, src_=src_: e.dma_start(out=dst_[:], in_=src_), w=[Bg])
        wglu = SB(es, "wglu", [128, 8, 1024], BF16); wgate = SB(es, "wgate", [128, 8, 2048], BF16)
        wao = SB(es, "wao", [128, 4, 1024], BF16); wco = SB(es, "wco", [128, 4, 1024], BF16)
        wout = SB(es, "wout", [128, 8, 1024], BF16); Bw = Buf()
        fw.dpl(lambda e: e.dma_start(out=wglu[:], in_=wview(w_in, 2120, 3144)), w=[Bw])
        fw.dpl(lambda e: e.dma_start(out=wgate[:, :, 0:1024], in_=wview(w_in, 3144, 4168)), w=[Bw])
        fw.dpl(lambda e: e.dma_start(out=wgate[:, :, 1024:2048], in_=wview(w_in, 4168, 5192)), w=[Bw])
        fw.dpl(lambda e: e.dma_start(out=wao[:], in_=wview(w_ao, 0, 1024)), w=[Bw])
        fw.dpl(lambda e: e.dma_start(out=wco[:], in_=wview(w_co, 0, 1024)), w=[Bw])
        fw.dpl(lambda e: e.dma_start(out=wout[:], in_=wview(w_out, 0, 1024)), w=[Bw])
        xt = [SB(es, f"xt2{i}", [128, D], F32) for i in range(3)]; Bxt = [Buf() for _ in range(3)]
        xht = [SB(es, f"xh2{i}", [32, D], F32) for i in range(3)]; Bxh = [Buf() for _ in range(3)]
        rt = rms_hT(es, "C"); rt2 = rms_hT(es, "Ch")
        hT = [SB(es, f"hT2{i}", [128, 8, 160], BF16) for i in range(3)]; BhT = [[Buf() for _ in range(8)] for _ in range(3)]; BhTh = [[Buf() for _ in range(8)] for _ in range(3)]
        sig = SB(es, "sig2", [128, 160], F32); Bsig = Buf()
        u = SB(es, "u2", [128, 4, 160], BF16); Bu = [Buf() for _ in range(4)]
        dgw = SB(es, "dgw", [128, 4, 31, 128], BF16); Bdgw = Buf()
        for cc in range(4):
            for k in range(31):
                fw.act(lambda e, cc=cc, k=k: e.activation(out=dgw[:, cc, k, :], in_=ident_b[:], func=AF.Copy, scale=cw[:, cc, k:k + 1]),
                       r=[Bg, B_const], w=[Bdgw])
        pC = PS(es, "pC2", [128, 4, 128]); BpC = Buf()
        yb = SB(es, "yb2", [128, 4, 128], F32); Byb = Buf()
        ysq = SB(es, "ysq2", [128, 4, 128], F32); Bysq = Buf()
        stat = SB(es, "stat2", [128, 4, 128], F32); Bstat = Buf()
        z = SB(es, "z2", [128, 4, 128], F32); Bz = Buf()
        sT = [SB(es, f"sT2{i}", [128, 4, 128], BF16) for i in range(2)]; BsT = [Buf(), Buf()]
        sg = SB(es, "sg2", [128, 2, 128], F32); Bsg = Buf()
        mm_ = SB(es, "mm2", [128, 2, 128], F32); Bmm = Buf()
        mixT = SB(es, "mixT", [128, 8, 128], BF16); BmixT = Buf()
        zer = SB(es, "zer2", [128, 512], F32)
        fw.pool(lambda e: e.memset(zer[:], 0.0), w=[Bg])
        for tsrc, c0 in ((peer_u, 0), (peer_v, D)):
            for ch in range(16):
                fw.dpl(lambda e, tsrc=tsrc, c0=c0, ch=ch: e.dma_start(
                    out=uv_bf[ch * 1024:(ch + 1) * 1024, c0:c0 + D].rearrange("(p r) d -> p r d", p=128),
                    in_=tsrc[ch * 1024:(ch + 1) * 1024, :].rearrange("(p r) d -> p r d", p=128)), w=[B_ubf])
        x1t = [SB(es, f"x1t{i}", [128, D], F32) for i in range(2)]; Bx1 = [Buf(), Buf()]
        attT_t = [SB(es, f"attTt{i}", [128, 4, 128], BF16) for i in range(2)]; BattT_t = [Buf(), Buf()]
        pTP = PS(es, "pTP2", [128, 8, 128], BF16); BpTP = Buf()
        pG = PS(es, "pG2", [128, 2, 160]); BpG = Buf()
        pSum = PS(es, "pSum2", [128, 2, 128]); BpSum = Buf()
        pM = [PS(es, f"pM2{i}", [128, 4, 128]) for i in range(2)]; BpM = [Buf(), Buf()]
        pX = [PS(es, f"pX2{i}", [128, 512]) for i in range(2)]; BpX = [Buf(), Buf()]
        B_x1s = Buf()
        mc = {"m": 0}

        def front_a1(j):
            s3 = j % 3
            fw.dsp(lambda e: e.dma_start(out=xt[s3][:], in_=xq[j * 128:(j + 1) * 128, :]), w=[Bxt[s3]])
            fw.dsp(lambda e: e.dma_start(out=xht[s3][:], in_=xh[j * 32:(j + 1) * 32, :]), w=[Bxh[s3]])
            emit_rms(rt2, xht[s3][:], Bxh[s3], 32, g_sb, Bg, pTP, BpTP, lambda c: hT[s3][:, c, 0:32], BhTh[s3], gbc=gabc2[:], hT_all=hT[s3][:, :, 0:32])
            emit_rms(rt, xt[s3][:], Bxt[s3], 128, g_sb, Bg, pTP, BpTP, lambda c: hT[s3][:, c, 32:160], BhT[s3], gbc=gabc2[:], hT_all=hT[s3][:, :, 32:160])

        def front_a2(j):
            s = j % 2
            s3 = j % 3
            srca = bass.AP(attTs_t, j * 128, [[NT, 128], [128 * NT, 4], [1, 128]])
            fw.dsp(lambda e: e.dma_start(out=attT_t[s][:], in_=srca), r=[B_attT[j]], w=[BattT_t[s]])
            for cc in range(4):
                for half in range(2):
                    fc = cc + 4 * half
                    for c in range(8):
                        fw.pe(lambda e, c=c, fc=fc, half=half: e.matmul(pG[:, half, :], wglu[:, c, fc * 128:(fc + 1) * 128], hT[s3][:, c, :],
                                                                        start=(c == 0 and half == 0), stop=(c == 7 and half == 1),
                                                                        skip_group_check=True), r=[BhT[s3][c], BhTh[s3][c], Bw], w=[BpG])
                fw.act(lambda e: e.activation(out=sig[:], in_=pG[:, 1, :], func=AF.Sigmoid), r=[BpG], w=[Bsig])
                fw.dve(lambda e, cc=cc: e.tensor_tensor(out=u[:, cc, :], in0=pG[:, 0, :], in1=sig[:], op=ALU.mult), r=[BpG, Bsig], w=[Bu[cc]])

        def conv_gen(j):
            ops = [(cc, k) for cc in range(4) for k in range(31)]
            per = (len(ops) + 7) // 8
            for chunk in range(8):
                for cc, k in ops[chunk * per:(chunk + 1) * per]:
                    fw.pe(lambda e, cc=cc, k=k: e.matmul(pC[:, cc, :], dgw[:, cc, k, :], u[:, cc, 2 + k:130 + k], start=(k == 0), stop=(k == 30),
                                                         skip_group_check=True), r=[Bu[cc], Bdgw], w=[BpC])
                yield

        def front_c(j):
            s = j % 2
            for cc in range(4):
                fw.act(lambda e, cc=cc: e.activation(out=yb[:, cc, :], in_=pC[:, cc, :], func=AF.Identity, bias=cb[:, cc:cc + 1]),
                       r=[BpC, Bg], w=[Byb])
            fw.act(lambda e: e.activation(out=ysq[:], in_=yb[:], func=AF.Square), r=[Byb], w=[Bysq])
            for cc in range(4):
                fw.pe(lambda e, cc=cc: e.matmul(pSum[:, 0, :], ones_f[:], yb[:, cc, :], start=(cc == 0), stop=False, skip_group_check=True),
                      r=[Byb, B_const], w=[BpSum])
            for cc in range(4):
                fw.pe(lambda e, cc=cc: e.matmul(pSum[:, 1, :], ones_f[:], ysq[:, cc, :], start=False, stop=(cc == 3), skip_group_check=True),
                      r=[Bysq, B_const], w=[BpSum])
            fw.dve(lambda e: e.tensor_scalar(out=stat[:, 0, :], in0=pSum[:, 0, :], scalar1=1.0 / 512, scalar2=None, op0=ALU.mult), r=[BpSum], w=[Bstat])
            fw.dve(lambda e: e.tensor_tensor(out=stat[:, 1, :], in0=stat[:, 0, :], in1=stat[:, 0, :], op=ALU.mult), r=[Bstat], w=[Bstat])
            fw.dve(lambda e: e.scalar_tensor_tensor(out=stat[:, 2, :], in0=pSum[:, 1, :], scalar=1.0 / 512, in1=stat[:, 1, :], op0=ALU.mult, op1=ALU.subtract),
                   r=[BpSum, Bstat], w=[Bstat])
            fw.act(lambda e: e.activation(out=stat[:, 2, :], in_=stat[:, 2, :], func=AF.Sqrt, bias=EPS), r=[Bstat], w=[Bstat])
            fw.dve(lambda e: e.reciprocal(out=stat[:, 3, :], in_=stat[:, 2, :]), r=[Bstat], w=[Bstat])
            fw.dve(lambda e: e.tensor_tensor(out=z[:], in0=yb[:], in1=xap(stat[:, 0, :], [[0, 4], [1, 128]]), op=ALU.subtract), r=[Byb, Bstat], w=[Bz])
            fw.dve(lambda e: e.tensor_tensor(out=z[:], in0=z[:], in1=xap(stat[:, 3, :], [[0, 4], [1, 128]]), op=ALU.mult), r=[Bz, Bstat], w=[Bz])
            for cc in range(4):
                fw.act(lambda e, cc=cc: e.activation(out=sT[s][:, cc, :], in_=z[:, cc, :], func=AF.Silu, scale=lg[:, cc:cc + 1], bias=lb[:, cc:cc + 1]),
                       r=[Bz, Bg], w=[BsT[s]])

        def back(j, cgen):
            s = j % 2
            s3 = j % 3
            for fc in range(8):
                if cgen is not None:
                    next(cgen, None)
                pm = mc["m"] % 2; mc["m"] += 1
                fw.act(lambda e, pm=pm: e.activation(out=xap(pM[pm][:], [[1, 512]]), in_=zer[:], func=AF.Copy), r=[Bg], w=[BpM[pm]])
                for gi in range(2):
                    for c in range(8):
                        fw.pe(lambda e, c=c, gi=gi, fc=fc, pm=pm: e.matmul(pM[pm][:, gi, :], wgate[:, c, gi * 1024 + fc * 128:gi * 1024 + (fc + 1) * 128],
                                                                           hT[s3][:, c, 32:160], start=False, stop=False, skip_group_check=True),
                              r=[BhT[s3][c], Bw], w=[BpM[pm]])
                for cc in range(4):
                    fw.pe(lambda e, cc=cc, fc=fc, pm=pm: e.matmul(pM[pm][:, 2, :], wao[:, cc, fc * 128:(fc + 1) * 128], attT_t[s][:, cc, :],
                                                                  start=False, stop=False, skip_group_check=True), r=[BattT_t[s], Bw], w=[BpM[pm]])
                for cc in range(4):
                    fw.pe(lambda e, cc=cc, fc=fc, pm=pm: e.matmul(pM[pm][:, 3, :], wco[:, cc, fc * 128:(fc + 1) * 128], sT[s][:, cc, :],
                                                                  start=False, stop=False, skip_group_check=True), r=[BsT[s], Bw], w=[BpM[pm]])
                for gi in range(2):
                    fw.act(lambda e, gi=gi, fc=fc, pm=pm: e.activation(out=sg[:, gi, :], in_=pM[pm][:, gi, :], func=AF.Sigmoid,
                                                                       bias=bg_sb[:, gi * 8 + fc:gi * 8 + fc + 1]), r=[BpM[pm], Bg], w=[Bsg])
                fw.dve(lambda e, pm=pm: e.tensor_tensor(out=mm_[:], in0=sg[:], in1=pM[pm][:, 2:4, :], op=ALU.mult), r=[Bsg, BpM[pm]], w=[Bmm])
                fw.dve(lambda e, fc=fc: e.tensor_tensor(out=mixT[:, fc, :], in0=mm_[:, 0, :], in1=mm_[:, 1, :], op=ALU.add), r=[Bmm], w=[BmixT])
            if cgen is not None:
                for _ in cgen:
                    pass

        def back_out(j):
            s = j % 2
            s3 = j % 3
            for half in range(2):
                for fc in range(8):
                    fw.pe(lambda e, fc=fc, half=half: e.matmul(pX[half][:], mixT[:, fc, :], wout[:, fc, half * 512:(half + 1) * 512],
                                                               start=(fc == 0), stop=(fc == 7)), r=[BmixT, Bw], w=[BpX[half]])
                fw.dve(lambda e, half=half: e.tensor_tensor(out=x1t[s][:, half * 512:(half + 1) * 512], in0=pX[half][:],
                                                            in1=xt[s3][:, half * 512:(half + 1) * 512], op=ALU.add), r=[BpX[half], Bxt[s3]], w=[Bx1[s]])
            fw.dsp(lambda e: e.dma_start(out=x1s[j * 128:(j + 1) * 128, :], in_=x1t[s][:]), r=[Bx1[s]], w=[B_x1s])

        front_a1(0)
        front_a2(0)
        front_a1(1)
        for _ in conv_gen(0):
            pass
        front_c(0)
        for j in range(NJ):
            if j + 1 < NJ:
                front_a2(j + 1)
                if j + 2 < NJ:
                    front_a1(j + 2)
                back(j, conv_gen(j + 1))
                front_c(j + 1)
                back_out(j)
            else:
                back(j, None)
                back_out(j)
        fw.barrier()

    with ExitStack() as es:
        gf_sb = SB(es, "gf_sb", [128, 8], F32); gp_sb = SB(es, "gp_sb", [128, 8], F32); Bg = Buf()
        gfbc = SB(es, "gfbc", [128, D], F32); io16 = SB(es, "io16", [128, 16], F32)
        for dst_, src_ in ((gf_sb, g_ffn), (gp_sb, g_ple), (gfbc, g_ffn_bc), (io16, iota16_d)):
            fw.dsp(lambda e, dst_=dst_, src_=src_: e.dma_start(out=dst_[:], in_=src_), w=[Bg])
        wpq = SB(es, "wpq", [128, 8, 2048], BF16); sk = SB(es, "sk", [128, 16, 128], BF16)
        wpg = SB(es, "wpg", [128, 8, 1024], BF16); wpp = SB(es, "wpp", [128, 2, 1024], BF16); Bw = Buf()
        fw.dpl(lambda e: e.dma_start(out=wpq[:, :, 0:1024], in_=wview(w_pq, 0, 1024)), w=[Bw])
        fw.dpl(lambda e: e.dma_start(out=wpq[:, :, 1024:2048], in_=wview(w_pq, 1024, 2048)), w=[Bw])
        fw.dpl(lambda e: e.dma_start(out=sk[:], in_=subk), w=[Bw])
        fw.dpl(lambda e: e.dma_start(out=wpg[:], in_=wview(w_pg, 0, 1024)), w=[Bw])
        fw.dpl(lambda e: e.dma_start(out=wpp[:], in_=wview(w_pp, 0, 1024)), w=[Bw])
        x1 = [SB(es, f"x1c{i}", [128, D], F32) for i in range(2)]; Bx1 = [Buf(), Buf()]
        pt_ = [SB(es, f"ptc{i}", [128, 256], F32) for i in range(2)]; Bpt = [Buf(), Buf()]
        rt = rms_hT(es, "P")
        h2T = SB(es, "h2T", [128, 8, 128], BF16); Bh2T = [Buf() for _ in range(8)]
        h2 = [SB(es, f"h2_{i}", [128, D], F32) for i in range(2)]; Bh2 = [Buf(), Buf()]
        q_sb = SB(es, "q_sb", [128, 2048], BF16); Bq = Buf()
        qTp = SB(es, "qTp", [128, 16, 128], BF16); BqT = Buf()
        sc = SB(es, "sc", [128, 16, 128], F32); Bsc = [Buf() for _ in range(16)]
        sv = SB(es, "sv", [128, 16, 16], F32); Bsv = [Buf() for _ in range(16)]
        si = SB(es, "si", [128, 16, 16], U32); Bsi = [Buf() for _ in range(16)]
        sif = SB(es, "sif", [128, 16, 16], F32); Bsif = Buf()
        cand = SB(es, "cand", [128, 8, 256], F32); Bcand = [Buf() for _ in range(8)]
        tops = SB(es, "tops", [128, 8, 16], F32); Btops = [Buf() for _ in range(8)]
        cj = SB(es, "cj", [128, 8, 16], U32); Bcj = [Buf() for _ in range(8)]
        cji = SB(es, "cji", [128, 2, 128], U32); cjf = SB(es, "cjf", [128, 2, 128], F32); Bcjf = Buf()
        ohx = SB(es, "ohx", [128, 128, 16], F32); Boh = Buf()
        abf = SB(es, "abf", [128, 2, 128], F32); Babf = Buf()
        eidx_f = SB(es, "eidx_f", [128, 128], F32); Beif = Buf()
        eidx = [SB(es, f"eidx{i}", [128, 128], I32) for i in range(2)]; Beidx = [Buf(), Buf()]
        ex = SB(es, "ex", [128, 8, 16], F32); zz = SB(es, "zz", [128, 16], F32); Bex = Buf()
        gate = [SB(es, f"gate{i}", [128, 128], F32) for i in range(2)]; Bgate = [Buf(), Buf()]
        dots = SB(es, "dots", [128, 128], F32); Bdots = [Buf() for _ in range(128)]
        tmpa = SB(es, "tmpa", [128, 128], F32); tmpb = SB(es, "tmpb", [128, 128], F32); Btmp = [Buf() for _ in range(32)]
        actv = SB(es, "actv", [128, 128], F32); Bactv = [Buf() for _ in range(32)]
        GS = 4
        NGB = 16
        gb = [SB(es, f"gb{i}", [128, 2 * D], BF16) for i in range(NGB)]; Bgb = [Buf() for _ in range(NGB)]
        NDG = 4
        dg = [SB(es, f"dg{i}", [128, 128], BF16) for i in range(NDG)]; Bdg = [Buf() for _ in range(NDG)]
        junk = SB(es, "junkp", [128, D], BF16); Bjunk = Buf()
        x2 = SB(es, "x2", [128, D], F32); Bx2 = Buf()
        h3T = SB(es, "h3T", [128, 8, 128], BF16); Bh3T = [Buf() for _ in range(8)]
        pb = SB(es, "pb", [128, 256], BF16); Bpb = Buf()
        pT = SB(es, "pT", [128, 2, 128], BF16); BpT = Buf()
        gsig = SB(es, "gsig", [128, D], F32); Bgsig = Buf()
        pTP = PS(es, "pTP3", [128, 8, 128], BF16); BpTP = Buf()
        pQ = [PS(es, f"pQ3{i}", [128, 512]) for i in range(2)]; BpQ = [Buf() for _ in range(2)]
        pA = [PS(es, f"pA3{i}", [128, 512]) for i in range(2)]; BpA = [Buf(), Buf()]
        pE = [PS(es, f"pE3{i}", [128, 512]) for i in range(2)]; BpE = [Buf(), Buf()]
        B_out = Buf()
        cn = {"g": 0, "d": 0}

        def route(j):
            s = j % 2
            fw.dsp(lambda e: e.dma_start(out=x1[s][:], in_=x1s[j * 128:(j + 1) * 128, :]), r=[B_x1s], w=[Bx1[s]])
            fw.dsp(lambda e: e.dma_start(out=pt_[s][:], in_=pq[j * 128:(j + 1) * 128, :]), w=[Bpt[s]])
            emit_rms(rt, x1[s][:], Bx1[s], 128, gf_sb, Bg, pTP, BpTP, lambda c: h2T[:, c, :], Bh2T, gbc=gfbc[:], h_f32=h2[s][:], Bh32=Bh2[s], hT_all=h2T[:])
            for n in range(4):
                pq_ = n % 2
                for c in range(8):
                    fw.pe(lambda e, c=c, n=n: e.matmul(pQ[pq_][:], h2T[:, c, :], wpq[:, c, n * 512:(n + 1) * 512], start=(c == 0), stop=(c == 7)),
                          r=[Bh2T[c], Bw], w=[BpQ[pq_]])
                fw.act(lambda e, n=n: e.activation(func=AF.Copy, out=q_sb[:, n * 512:(n + 1) * 512], in_=pQ[pq_][:]), r=[BpQ[pq_]], w=[Bq])
            for half in range(2):
                for i in range(8):
                    hc = half * 8 + i
                    fw.pe(lambda e, i=i, hc=hc: e.transpose(out=pTP[:, i, :], in_=q_sb[:, hc * 128:(hc + 1) * 128], identity=ident_b[:]),
                          r=[Bq, B_const], w=[BpTP])
                fw.act(lambda e, half=half: e.activation(func=AF.Copy, out=qTp[:, half * 8:(half + 1) * 8, :], in_=pTP[:]), r=[BpTP], w=[BqT])
            for n in range(4):
                pq_ = n % 2
                for i in range(4):
                    hc = n * 4 + i
                    fw.pe(lambda e, i=i, hc=hc: e.matmul(pQ[pq_][:, i * 128:(i + 1) * 128], qTp[:, hc, :], sk[:, hc, :], start=True, stop=True,
                                                         skip_group_check=True), r=[BqT, Bw], w=[BpQ[pq_]])
                fw.act(lambda e, n=n: e.activation(func=AF.Copy, out=xap(sc[:, n * 4, :], [[1, 512]]), in_=pQ[pq_][:]), r=[BpQ[pq_]],
                       w=[Bsc[n * 4 + i] for i in range(4)])
            yield
            for rnd in range(2):
                o = rnd * 8
                for hc in range(16):
                    fw.dve(lambda e, hc=hc, o=o: e.max(out=sv[:, hc, o:o + 8], in_=sc[:, hc, :]), r=[Bsc[hc]], w=[Bsv[hc]])
                for hc in range(16):
                    fw.dve(lambda e, hc=hc, o=o: e.max_index(out=si[:, hc, o:o + 8], in_max=sv[:, hc, o:o + 8], in_values=sc[:, hc, :]),
                           r=[Bsc[hc], Bsv[hc]], w=[Bsi[hc]])
                if rnd == 0:
                    for hc in range(16):
                        fw.dve(lambda e, hc=hc: e.match_replace(out=sc[:, hc, :], in_to_replace=sv[:, hc, 0:8], in_values=sc[:, hc, :], imm_value=NEG_SEL),
                               r=[Bsv[hc], Bsc[hc]], w=[Bsc[hc]])
            fw.dve(lambda e: e.tensor_copy(out=sif[:], in_=si[:]), r=Bsi, w=[Bsif])
            yield
            for h in range(8):
                a0 = sv[:, 2 * h, :]; b0 = sv[:, 2 * h + 1, :]
                fw.dve(lambda e, h=h, a0=a0, b0=b0: e.tensor_tensor(out=xap(cand[:, h, :], [[16, 16], [1, 16]]), in0=xap(a0, [[1, 16], [0, 16]]),
                                                                    in1=xap(b0, [[0, 16], [1, 16]]), op=ALU.add),
                       r=[Bsv[2 * h], Bsv[2 * h + 1]], w=[Bcand[h]])
            for rnd in range(2):
                o = rnd * 8
                for h in range(8):
                    fw.dve(lambda e, h=h, o=o: e.max(out=tops[:, h, o:o + 8], in_=cand[:, h, :]), r=[Bcand[h]], w=[Btops[h]])
                for h in range(8):
                    fw.dve(lambda e, h=h, o=o: e.max_index(out=cj[:, h, o:o + 8], in_max=tops[:, h, o:o + 8], in_values=cand[:, h, :]),
                           r=[Bcand[h], Btops[h]], w=[Bcj[h]])
                if rnd == 0:
                    for h in range(8):
                        fw.dve(lambda e, h=h: e.match_replace(out=cand[:, h, :], in_to_replace=tops[:, h, 0:8], in_values=cand[:, h, :], imm_value=NEG_SEL),
                               r=[Btops[h], Bcand[h]], w=[Bcand[h]])
            yield
            fw.dve(lambda e: e.tensor_tensor(out=ex[:], in0=tops[:], in1=xap(tops[:, 0, 0:1], [[16, 8], [0, 16]]), op=ALU.subtract), r=Btops, w=[Bex])
            fw.act(lambda e: e.activation(out=ex[:], in_=ex[:], func=AF.Exp), r=[Bex], w=[Bex])
            fw.dve(lambda e: e.tensor_reduce(out=zz[:, 0:8], in_=ex[:], axis=AX.X, op=ALU.add), r=[Bex], w=[Bex])
            fw.dve(lambda e: e.reciprocal(out=zz[:, 8:16], in_=zz[:, 0:8]), r=[Bex], w=[Bex])
            fw.dve(lambda e: e.tensor_tensor(out=xap(gate[s][:], [[16, 8], [1, 16]]), in0=ex[:], in1=xap(zz[:, 8:16], [[1, 8], [0, 16]]), op=ALU.mult),
                   r=[Bex], w=[Bgate[s]])
            cj_flat = xap(cj[:], [[1, 128]])
            fw.dve(lambda e: e.tensor_single_scalar(out=cji[:, 0, :], in_=cj_flat, scalar=4, op=ALU.logical_shift_right), r=Bcj, w=[Bcjf])
            fw.dve(lambda e: e.tensor_single_scalar(out=cji[:, 1, :], in_=cj_flat, scalar=15, op=ALU.bitwise_and), r=Bcj, w=[Bcjf])
            fw.dve(lambda e: e.tensor_copy(out=cjf[:], in_=cji[:]), r=[Bcjf], w=[Bcjf])
            for ab in range(2):
                yield
                fw.dve(lambda e, ab=ab: e.tensor_tensor(out=ohx[:], in0=xap(io16[:], [[0, 128], [1, 16]]), in1=xap(cjf[:, ab, :], [[1, 128], [0, 16]]),
                                                        op=ALU.is_equal), r=[Bg, Bcjf], w=[Boh])
                fw.dve(lambda e, ab=ab: e.tensor_tensor(out=xap(ohx[:], [[256, 8], [16, 16], [1, 16]]), in0=xap(ohx[:], [[256, 8], [16, 16], [1, 16]]),
                                                        in1=xap(sif[:, ab, :], [[32, 8], [0, 16], [1, 16]]), op=ALU.mult), r=[Boh, Bsif], w=[Boh])
                fw.dve(lambda e, ab=ab: e.tensor_reduce(out=abf[:, ab, :], in_=ohx[:], axis=AX.X, op=ALU.add), r=[Boh], w=[Babf])
            fw.dve(lambda e: e.scalar_tensor_tensor(out=eidx_f[:], in0=abf[:, 0, :], scalar=128.0, in1=abf[:, 1, :], op0=ALU.mult, op1=ALU.add),
                   r=[Babf], w=[Beif])
            fw.dve(lambda e: e.tensor_copy(out=eidx[s][:], in_=eidx_f[:]), r=[Beif], w=[Beidx[s]])

        def experts(j, rgen):
            s = j % 2
            slot_buf = {}

            def grp_dots(g):
                for i in range(GS):
                    sl = g * GS + i
                    b = cn["g"] % NGB; cn["g"] += 1
                    slot_buf[sl] = b
                    fw.dpl(lambda e, sl=sl, b=b: e.indirect_dma_start(out=gb[b][:], out_offset=None, in_=uv_bf,
                                                                      in_offset=bass.IndirectOffsetOnAxis(ap=eidx[s][:, sl:sl + 1], axis=0)),
                           r=[Beidx[s], B_ubf], w=[Bgb[b]])
                    fw.dve(lambda e, sl=sl, b=b: e.scalar_tensor_tensor(out=junk[:], in0=gb[b][:, 0:D], scalar=1.0, in1=h2[s][:], op0=ALU.mult, op1=ALU.mult,
                                                                        accum_out=dots[:, sl:sl + 1]), r=[Bgb[b], Bh2[s]], w=[Bdots[sl]])

            def grp_pre(g):
                c = slice(g * GS, (g + 1) * GS)
                fw.act(lambda e: e.activation(out=tmpa[:, c], in_=dots[:, c], func=AF.Gelu_apprx_tanh), r=Bdots[g * GS:(g + 1) * GS], w=[Btmp[g]])

            def grp_post(g):
                c = slice(g * GS, (g + 1) * GS)
                fw.dve(lambda e: e.tensor_tensor(out=actv[:, c], in0=tmpa[:, c], in1=gate[s][:, c], op=ALU.mult), r=[Btmp[g], Bgate[s]], w=[Bactv[g]])
                for i in range(GS):
                    sl = g * GS + i
                    b = slot_buf[sl]
                    k = cn["d"] % NDG; cn["d"] += 1
                    fw.act(lambda e, sl=sl, k=k: e.activation(out=dg[k][:], in_=ident_b[:], func=AF.Copy, scale=actv[:, sl:sl + 1]),
                           r=[Bactv[g], B_const], w=[Bdg[k]])
                    for half in range(2):
                        fw.pe(lambda e, sl=sl, b=b, k=k, half=half: e.matmul(pA[half][:], dg[k][:], gb[b][:, D + half * 512:D + (half + 1) * 512],
                                                                             start=(sl == 0), stop=(sl == 127)), r=[Bdg[k], Bgb[b]], w=[BpA[half]])

            for g in range(32):
                grp_dots(g)
                grp_pre(g)
                if g >= 1:
                    grp_post(g - 1)
                if rgen is not None and g % 4 == 3:
                    next(rgen, None)
            grp_post(31)
            if rgen is not None:
                for _ in rgen:
                    pass
            for half in range(2):
                fw.dve(lambda e, half=half: e.tensor_tensor(out=x2[:, half * 512:(half + 1) * 512], in0=pA[half][:],
                                                            in1=x1[s][:, half * 512:(half + 1) * 512], op=ALU.add), r=[BpA[half], Bx1[s]], w=[Bx2])
            emit_rms(rt, x2[:], Bx2, 128, gp_sb, Bg, pTP, BpTP, lambda c: h3T[:, c, :], Bh3T)
            fw.act(lambda e: e.activation(func=AF.Copy, out=pb[:], in_=pt_[s][:]), r=[Bpt[s]], w=[Bpb])
            for c2 in range(2):
                fw.pe(lambda e, c2=c2: e.transpose(out=pTP[:, c2, :], in_=pb[:, c2 * 128:(c2 + 1) * 128], identity=ident_b[:]), r=[Bpb, B_const], w=[BpTP])
            fw.act(lambda e: e.activation(func=AF.Copy, out=pT[:], in_=pTP[:, 0:2, :]), r=[BpTP], w=[BpT])
            for half in range(2):
                for c in range(8):
                    fw.pe(lambda e, c=c, half=half: e.matmul(pE[0][:], h3T[:, c, :], wpg[:, c, half * 512:(half + 1) * 512], start=(c == 0), stop=(c == 7)),
                          r=[Bh3T[c], Bw], w=[BpE[0]])
                fw.act(lambda e, half=half: e.activation(out=gsig[:, half * 512:(half + 1) * 512], in_=pE[0][:], func=AF.Sigmoid), r=[BpE[0]], w=[Bgsig])
                for c2 in range(2):
                    fw.pe(lambda e, c2=c2, half=half: e.matmul(pE[1][:], pT[:, c2, :], wpp[:, c2, half * 512:(half + 1) * 512], start=(c2 == 0), stop=(c2 == 1)),
                          r=[BpT, Bw], w=[BpE[1]])
                fw.dve(lambda e, half=half: e.tensor_tensor(out=gsig[:, half * 512:(half + 1) * 512], in0=gsig[:, half * 512:(half + 1) * 512],
                                                            in1=pE[1][:], op=ALU.mult), r=[Bgsig, BpE[1]], w=[Bgsig])
            fw.dve(lambda e: e.tensor_tensor(out=gsig[:], in0=gsig[:], in1=x2[:], op=ALU.add), r=[Bgsig, Bx2], w=[Bgsig])
            fw.dsp(lambda e: e.dma_start(out=out_d[j * 128:(j + 1) * 128, :], in_=gsig[:]), r=[Bgsig], w=[B_out])

        for _ in route(0):
            pass
        for j in range(NJ):
            experts(j, route(j + 1) if j + 1 < NJ else None)
        fw.barrier()
    es_all.close()
    return nc


def _t5_bucket(rel):
    rel = np.asarray(rel, dtype=np.int64)
    half, max_exact = 16, 8
    ret = np.where(rel > 0, half, 0)
    n = np.abs(rel)
    nf = np.maximum(n, 1).astype(np.float32)
    large = max_exact + (np.log(nf / np.float32(max_exact)) / np.float32(math.log(1024 / max_exact)) * np.float32(half - max_exact)).astype(np.int32)
    large = np.minimum(large, half - 1)
    return ret + np.where(n < max_exact, n, large)


_PROG = None


def kernel(x, p, rel_bias, attn_norm_g, w_in, b_gate, q_norm_g, k_norm_g, w_att_out,
           conv_w, conv_b, conv_ln_g, conv_ln_b, w_conv_out, w_out, ffn_norm_g,
           w_peer_q, peer_sub_keys, peer_u, peer_v, ple_norm_g, w_ple_gate, w_ple_proj):
    global _PROG
    f = np.float32
    x = np.asarray(x, f); p = np.asarray(p, f)
    c128 = lambda v, n: np.ascontiguousarray(np.asarray(v, f).reshape(n, 128).T)
    shared = {
        "w_in": np.ascontiguousarray(np.asarray(w_in, f)[0]),
        "rel_bias": np.ascontiguousarray(np.asarray(rel_bias, f)),
        "g_attn": c128(attn_norm_g[0], 8), "g_ffn": c128(ffn_norm_g[0], 8), "g_ple": c128(ple_norm_g[0], 8),
        "g_ffn_bc": np.ascontiguousarray(np.broadcast_to(np.asarray(ffn_norm_g, f)[0][None, :], (128, D))),
        "g_attn_bc": np.ascontiguousarray(np.broadcast_to(np.asarray(attn_norm_g, f)[0][None, :], (128, D))),
        "gq_bc": np.ascontiguousarray(np.broadcast_to(np.tile(np.asarray(q_norm_g, f)[0], 8)[None, :], (128, 512))),
        "gk_bc": np.ascontiguousarray(np.broadcast_to(np.tile(np.asarray(k_norm_g, f)[0], 8)[None, :], (128, 512))),
        "bgate": c128(b_gate[0], 16),
        "w_att_out": np.ascontiguousarray(np.asarray(w_att_out, f)[0]),
        "w_conv_out": np.ascontiguousarray(np.asarray(w_conv_out, f)[0]),
        "w_out": np.ascontiguousarray(np.asarray(w_out, f)[0]),
        "convw": np.ascontiguousarray(np.asarray(conv_w, f)[0][:, 0, :].reshape(31, 4, 128).transpose(2, 1, 0)),
        "convb": c128(conv_b[0], 4), "lng": c128(conv_ln_g[0], 4), "lnb": c128(conv_ln_b[0], 4),
        "w_peer_q": np.ascontiguousarray(np.asarray(w_peer_q, f)[0]),
        "subk": np.ascontiguousarray(np.asarray(peer_sub_keys, f)[0].reshape(16, 128, 128).transpose(2, 0, 1)),
        "peer_u": np.ascontiguousarray(np.asarray(peer_u, f)[0]),
        "peer_v": np.ascontiguousarray(np.asarray(peer_v, f)[0]),
        "w_ple_gate": np.ascontiguousarray(np.asarray(w_ple_gate, f)[0]),
        "w_ple_proj": np.ascontiguousarray(np.asarray(w_ple_proj, f)[0]),
        "ident": np.eye(128, dtype=f),
        "anti": np.ascontiguousarray(np.eye(128, dtype=f)[::-1]),
        "iota16": np.ascontiguousarray(np.broadcast_to(np.arange(16, dtype=f)[None, :], (128, 16))),
        "pw2": np.ascontiguousarray(np.broadcast_to((2.0 ** -(np.arange(32) + 1.0)).astype(f)[None, :], (128, 32))),
    }
    tt = np.arange(128)
    halfmask = np.where((tt[None, :] // 64) <= (tt[:, None] // 64), 0.0, -1.0e9).astype(f)
    in_maps = []
    for core in range(8):
        b, c = core // 2, core % 2
        xb = x[b].reshape(64, 128, D)
        own = xb[c::2]
        halo = np.zeros((NJ, 32, D), f)
        for j in range(NJ):
            blk = 2 * j + c
            if blk > 0:
                halo[j] = xb[blk - 1][96:128]
        idx = np.arange(1536)
        rel = 128 * (1 - c) + 127 - idx
        ohm = (np.arange(32)[:, None] == _t5_bucket(rel)[None, :]).astype(f)
        vm = np.zeros((128, 256), f)
        if c == 0:
            vm[:, 0:128] = halfmask; vm[:, 128:256] = -1.0e9
        else:
            vm[:, 128:256] = halfmask
        m = dict(shared)
        m.update({
            "xs": np.ascontiguousarray(x[b]),
            "xq": np.ascontiguousarray(own.reshape(NT, D)),
            "xh": np.ascontiguousarray(halo.reshape(NJ * 32, D)),
            "pq": np.ascontiguousarray(p[0, b].reshape(64, 128, 256)[c::2].reshape(NT, 256)),
            "oh": ohm, "vm": vm,
        })
        in_maps.append(m)
    if _PROG is None:
        _PROG = build_program()
    res = run_bass_kernel_spmd(_PROG, in_maps, core_ids=list(range(8)))
    out = np.zeros((4, 64, 128, D), f)
    for core in range(8):
        b, c = core // 2, core % 2
        out[b, c::2] = np.asarray(res.results[core]["out"], f).reshape(NJ, 128, D)
    return out.reshape(4, S, D)
```
